# Optimizing a Trainium2 kernel written in Bass

```python
import math
import jax, jax.numpy as jnp
from jax import lax
import numpy as np

D_MODEL = 2048
BATCH = 1
SEQ = 8192
DEPTH = 1

CHUNK = 64
EPS = 1e-6
RW_HEADS = 16
RW_HEAD_DIM = 64
RW_WIDTH = RW_HEADS * RW_HEAD_DIM
RW_DECAY_RANK = 64
RW_ICL_RANK = 64
RW_GATE_RANK = 160
RW_GN_EPS = 64e-5
RW_SPLITS = (RW_WIDTH, RW_WIDTH, RW_WIDTH, RW_DECAY_RANK, RW_ICL_RANK, RW_GATE_RANK)
RW_IN = 3 * RW_WIDTH + RW_DECAY_RANK + RW_ICL_RANK + RW_GATE_RANK
SA_HEADS = 16
SA_HEAD_DIM = 64
SA_WIDTH = SA_HEADS * SA_HEAD_DIM
SA_SCALE = SA_HEAD_DIM ** -0.5
KV_RANK = 256
IDX_HEADS = 8
IDX_DIM = 64
IDX_SCALE = (IDX_HEADS * IDX_DIM) ** -0.5
TOPK_MAX = 256
Q_BLOCK = 128
REL_BUCKETS = 32
REL_MAX_DIST = 1024
N_BRANCHES = 2
IN_SPLITS = (RW_IN, SA_WIDTH, KV_RANK, IDX_HEADS * IDX_DIM, IDX_DIM, IDX_HEADS, N_BRANCHES * D_MODEL)
IN_TOTAL = RW_IN + SA_WIDTH + KV_RANK + IDX_HEADS * IDX_DIM + IDX_DIM + IDX_HEADS + N_BRANCHES * D_MODEL
PEER_HEADS = 8
PEER_KEYS = 128
PEER_EXPERTS = PEER_KEYS * PEER_KEYS
PEER_QDIM = 256
PEER_HALF = PEER_QDIM // 2
PEER_TOPK = 16
PEER_BLOCK = 128

kernel_name = 'hybrid_rwkv7_dsa_peer'


def _split(x, sizes):
    idx = []
    acc = 0
    for s in sizes[:-1]:
        acc += s
        idx.append(acc)
    return jnp.split(x, idx, axis=-1)


def _rmsnorm(x, g):
    x32 = x.astype(jnp.float32)
    y = x32 * lax.rsqrt(jnp.mean(x32 * x32, axis=-1, keepdims=True) + EPS)
    return (y * g.astype(jnp.float32)).astype(x.dtype)


def _rel_bucket(rel):
    nb = REL_BUCKETS // 2
    max_exact = nb // 2
    ret = (rel > 0).astype(jnp.int32) * nb
    n = jnp.abs(rel)
    large = max_exact + (jnp.log(jnp.maximum(n, 1).astype(jnp.float32) / max_exact)
                         / math.log(REL_MAX_DIST / max_exact) * (nb - max_exact)).astype(jnp.int32)
    large = jnp.minimum(large, nb - 1)
    return ret + jnp.where(n < max_exact, n, large)


def _rwkv7_time_mix(p, shift_mix, w0, w_decay_up, a0, w_icl_up, w_gate_up, k_k, k_a, r_k, ln_x_w, ln_x_b):
    B, T, _ = p.shape
    H, N = RW_HEADS, RW_HEAD_DIM
    f32 = jnp.float32
    prev = jnp.pad(p, ((0, 0), (1, 0), (0, 0)))[:, :T]
    z = p + (prev - p) * shift_mix
    r, k, v, zw, za, zg = _split(z, RW_SPLITS)
    w_log = -jax.nn.softplus(-(w0 + jnp.tanh(zw) @ w_decay_up).astype(f32)) - 0.5
    decay = jnp.exp(-jnp.exp(w_log))
    a = jax.nn.sigmoid((a0 + za @ w_icl_up).astype(f32))
    g = jax.nn.sigmoid(zg) @ w_gate_up
    kf = k.astype(f32)
    kk = (kf * k_k.astype(f32)).reshape(B, T, H, N)
    kk = kk / jnp.maximum(jnp.linalg.norm(kk, axis=-1, keepdims=True), 1e-12)
    k4 = (kf * (1.0 + (a - 1.0) * k_a.astype(f32))).reshape(B, T, H, N)
    r4 = r.astype(f32).reshape(B, T, H, N)
    v4 = v.astype(f32).reshape(B, T, H, N)
    a4 = a.reshape(B, T, H, N)
    w4 = decay.reshape(B, T, H, N)

    def step(S, inp):
        r_t, k_t, v_t, kk_t, a_t, w_t = inp
        sa = jnp.einsum('bhvk,bhk->bhv', S, -kk_t)
        S = (S * w_t[:, :, None, :] + sa[..., None] * (kk_t * a_t)[:, :, None, :]
             + v_t[..., None] * k_t[:, :, None, :])
        return S, jnp.einsum('bhvk,bhk->bhv', S, r_t)

    xs = tuple(jnp.moveaxis(t, 1, 0) for t in (r4, k4, v4, kk, a4, w4))
    _, ys = lax.scan(step, jnp.zeros((B, H, N, N), f32), xs)
    y = jnp.moveaxis(ys, 0, 1)
    mean = jnp.mean(y, axis=-1, keepdims=True)
    var = jnp.mean(jnp.square(y - mean), axis=-1, keepdims=True)
    y = ((y - mean) * lax.rsqrt(var + RW_GN_EPS)).reshape(B, T, RW_WIDTH)
    y = y * ln_x_w.astype(f32) + ln_x_b.astype(f32)
    bonus = jnp.sum(r4 * k4 * r_k.astype(f32), axis=-1, keepdims=True) * v4
    y = (y + bonus.reshape(B, T, RW_WIDTH)) * g.astype(f32)
    return y.astype(p.dtype)


def _dsa_attention(q, c_kv, iq, ik, iw, kv_norm_g, w_uk, w_uv, rel_bias):
    B, T, _ = q.shape
    topk = min(TOPK_MAX, T // 4)
    f32 = jnp.float32
    c = _rmsnorm(c_kv, kv_norm_g)
    q_lat = jnp.einsum('bthd,hrd->bthr', q.reshape(B, T, SA_HEADS, SA_HEAD_DIM), w_uk)
    iq = iq.reshape(B, T, IDX_HEADS, IDX_DIM)
    iw = iw * IDX_SCALE
    key_chunk = jnp.arange(T) // CHUNK

    def block(i):
        s0 = i * Q_BLOCK
        qi = lax.dynamic_slice_in_dim(iq, s0, Q_BLOCK, axis=1)
        wi = lax.dynamic_slice_in_dim(iw, s0, Q_BLOCK, axis=1)
        ql = lax.dynamic_slice_in_dim(q_lat, s0, Q_BLOCK, axis=1)
        tq = s0 + jnp.arange(Q_BLOCK)
        score = jnp.einsum('bqhs,bqh->bqs', jax.nn.relu(jnp.einsum('bqhd,bsd->bqhs', qi, ik)), wi).astype(f32)
        allowed = key_chunk[None, :] <= (tq // CHUNK)[:, None]
        score = jnp.where(allowed[None], score, -jnp.inf)
        top_s, idx = lax.top_k(score, topk)
        valid = jnp.isfinite(top_s)
        c_sel = jax.vmap(lambda cb, ib: cb[ib])(c, idx)
        logits = jnp.einsum('bqhr,bqkr->bqhk', ql, c_sel).astype(f32) * SA_SCALE
        bias = rel_bias[_rel_bucket(idx - tq[None, :, None])]
        logits = logits + jnp.transpose(bias, (0, 1, 3, 2)).astype(f32)
        logits = jnp.where(valid[:, :, None, :], logits, -jnp.inf)
        pr = jax.nn.softmax(logits, axis=-1).astype(c.dtype)
        o_lat = jnp.einsum('bqhk,bqkr->bqhr', pr, c_sel)
        o = jnp.einsum('bqhr,hrd->bqhd', o_lat, w_uv)
        return o.reshape(B, Q_BLOCK, SA_WIDTH)

    out = lax.map(block, jnp.arange(T // Q_BLOCK))
    return jnp.moveaxis(out, 0, 1).reshape(B, T, SA_WIDTH)


def _peer_ffn(h, w_query, sub_keys, expert_u, expert_v):
    B, T, D = h.shape
    q = (h @ w_query).reshape(B, T, PEER_HEADS, 2, PEER_HALF)
    s = jnp.einsum('bthcq,hcnq->bthcn', q, sub_keys).astype(jnp.float32)
    s1, i1 = lax.top_k(s[..., 0, :], PEER_TOPK)
    s2, i2 = lax.top_k(s[..., 1, :], PEER_TOPK)
    cand = (s1[..., :, None] + s2[..., None, :]).reshape(B, T, PEER_HEADS, PEER_TOPK * PEER_TOPK)
    cand_id = (i1[..., :, None] * PEER_KEYS + i2[..., None, :]).reshape(B, T, PEER_HEADS, PEER_TOPK * PEER_TOPK)
    top_s, pos = lax.top_k(cand, PEER_TOPK)
    eid = jnp.take_along_axis(cand_id, pos, axis=-1)
    gate = jax.nn.softmax(top_s, axis=-1).astype(h.dtype)
    nb = (B * T) // PEER_BLOCK
    hb = h.reshape(nb, PEER_BLOCK, D)
    eb = eid.reshape(nb, PEER_BLOCK, PEER_HEADS, PEER_TOPK)
    gb = gate.reshape(nb, PEER_BLOCK, PEER_HEADS, PEER_TOPK)

    def blk(args):
        hx, e, gt = args
        act = jax.nn.gelu(jnp.einsum('nd,nhkd->nhk', hx, expert_u[e]), approximate=False)
        return jnp.einsum('nhk,nhkd->nd', gt * act, expert_v[e])

    return lax.map(blk, (hb, eb, gb)).reshape(B, T, D)


def setup_inputs(seed: int = 0) -> dict:
    key = jax.random.key(seed)
    ks = iter(jax.random.split(key, 40))
    L, D = DEPTH, D_MODEL
    nrm = lambda shape, scale: jax.random.normal(next(ks), shape, jnp.float32) * scale
    uni = lambda shape, lo, hi: jax.random.uniform(next(ks), shape, jnp.float32, lo, hi)
    gain = lambda shape: 1.0 + nrm(shape, 0.02)
    return {
        'x': nrm((BATCH, SEQ, D), 1.0),
        'norm_mix_g': gain((L, D)),
        'w_in': nrm((L, D, IN_TOTAL), D ** -0.5),
        'shift_mix': uni((L, RW_IN), 0.0, 1.0),
        'w0': uni((L, RW_WIDTH), -4.0, 0.0),
        'w_decay_up': nrm((L, RW_DECAY_RANK, RW_WIDTH), 0.3 * RW_DECAY_RANK ** -0.5),
        'a0': nrm((L, RW_WIDTH), 0.3),
        'w_icl_up': nrm((L, RW_ICL_RANK, RW_WIDTH), RW_ICL_RANK ** -0.5),
        'w_gate_up': nrm((L, RW_GATE_RANK, RW_WIDTH), RW_GATE_RANK ** -0.5),
        'k_k': 0.85 + nrm((L, RW_WIDTH), 0.05),
        'k_a': 1.0 + nrm((L, RW_WIDTH), 0.05),
        'r_k': nrm((L, RW_HEADS, RW_HEAD_DIM), 0.1),
        'ln_x_w': gain((L, RW_WIDTH)),
        'ln_x_b': nrm((L, RW_WIDTH), 0.02),
        'kv_norm_g': gain((L, KV_RANK)),
        'w_uk': nrm((L, SA_HEADS, KV_RANK, SA_HEAD_DIM), KV_RANK ** -0.5),
        'w_uv': nrm((L, SA_HEADS, KV_RANK, SA_HEAD_DIM), KV_RANK ** -0.5),
        'rel_bias': nrm((REL_BUCKETS, SA_HEADS), 0.5),
        'w_branch_rwkv': nrm((L, RW_WIDTH, D), RW_WIDTH ** -0.5),
        'w_branch_dsa': nrm((L, SA_WIDTH, D), SA_WIDTH ** -0.5),
        'w_out': nrm((L, D, D), D ** -0.5),
        'norm_ffn_g': gain((L, D)),
        'w_peer_query': nrm((L, D, PEER_HEADS * PEER_QDIM), D ** -0.5),
        'peer_sub_keys': nrm((L, PEER_HEADS, 2, PEER_KEYS, PEER_HALF), PEER_HALF ** -0.5),
        'peer_u': nrm((L, PEER_EXPERTS, D), D ** -0.5),
        'peer_v': nrm((L, PEER_EXPERTS, D), 0.25),
        'norm_final_g': gain((D,)),
    }


def reference(x, norm_mix_g, w_in, shift_mix, w0, w_decay_up, a0, w_icl_up, w_gate_up, k_k, k_a, r_k,
              ln_x_w, ln_x_b, kv_norm_g, w_uk, w_uv, rel_bias, w_branch_rwkv, w_branch_dsa, w_out,
              norm_ffn_g, w_peer_query, peer_sub_keys, peer_u, peer_v, norm_final_g):
    h = x
    for l in range(DEPTH):
        xn = _rmsnorm(h, norm_mix_g[l])
        p_rw, q, c_kv, iq, ik, iw, gates = _split(xn @ w_in[l], IN_SPLITS)
        y_a = _rwkv7_time_mix(p_rw, shift_mix[l], w0[l], w_decay_up[l], a0[l], w_icl_up[l], w_gate_up[l],
                              k_k[l], k_a[l], r_k[l], ln_x_w[l], ln_x_b[l]) @ w_branch_rwkv[l]
        y_b = _dsa_attention(q, c_kv, iq, ik, iw, kv_norm_g[l], w_uk[l], w_uv[l], rel_bias) @ w_branch_dsa[l]
        g_a, g_b = jnp.split(jax.nn.sigmoid(gates), N_BRANCHES, axis=-1)
        h = h + (g_a * y_a + g_b * y_b) @ w_out[l]
        h = h + _peer_ffn(_rmsnorm(h, norm_ffn_g[l]), w_peer_query[l], peer_sub_keys[l], peer_u[l], peer_v[l])
    return _rmsnorm(h, norm_final_g)
```

```python
import contextlib
import math
import numpy as np
import concourse.bass as bass
import concourse.mybir as mybir
from concourse.bass_utils import run_bass_kernel_spmd

F32 = mybir.dt.float32
BF16 = mybir.dt.bfloat16
ALU = mybir.AluOpType
AF = mybir.ActivationFunctionType
AX = mybir.AxisListType

WRITE_KEYS = ("out", "accum_out", "out_max", "out_indices")

NCORES = 8
D = 2048
T = 8192
TB = 256
NBLK = T // TB
EPS = 1e-6
GN_EPS = 64e-5
W1C = 992
NVEC = 2304
IDX_SCALE = 512 ** -0.5
SA_SCALE = 64 ** -0.5


class Eng:
    def __init__(self, name, h, sem):
        self.name, self.h, self.sem, self.count = name, h, sem, 0
        self.known = {}


class Builder:
    def __init__(self, nc):
        self.nc = nc
        self.es = contextlib.ExitStack()
        self.reg = {}
        self.engs = {}
        for name, h in (("pe", nc.tensor), ("act", nc.scalar), ("dve", nc.vector),
                        ("pool", nc.gpsimd), ("sp", nc.sync)):
            sem = self.es.enter_context(nc.semaphore("s_" + name))
            self.engs[name] = Eng(name, h, sem)
        self.dsem = {}
        self.n_ins = 0
        self.stack = [self.es]

    def push(self):
        st = contextlib.ExitStack()
        self.stack.append(st)

    def pop(self):
        self.barrier()
        self.stack.pop().close()

    def sb(self, name, shape, dt=F32):
        used = self.__dict__.setdefault("_names", {})
        n = used.get(name, 0)
        used[name] = n + 1
        if n:
            name = "%s_v%d" % (name, n)
        return self.stack[-1].enter_context(self.nc.sbuf_tensor(name, list(shape), dt))

    def ps(self, name, shape, dt=F32):
        return self.es.enter_context(self.nc.psum_tensor(name, list(shape), dt))

    def dram(self, name, shape, dt=F32, kind="Internal"):
        return self.nc.dram_tensor(name, list(shape), dt, kind=kind)

    def _key(self, ap):
        return ap.tensor.name

    def _deps(self, reads, writes):
        deps = []
        for k in reads:
            r = self.reg.get(k)
            if r and r[0]:
                deps.append(r[0])
        for k in writes:
            r = self.reg.get(k)
            if r:
                if r[0]:
                    deps.append(r[0])
                deps.extend(r[1])
        return deps

    def _commit(self, tok, reads, writes):
        for k in reads:
            r = self.reg.setdefault(k, [None, []])
            r[1].append(tok)
            if len(r[1]) > 12:
                best = {}
                for s, v in r[1]:
                    if v > best.get(id(s), (None, 0))[1]:
                        best[id(s)] = (s, v)
                r[1] = list(best.values())
        for k in writes:
            self.reg[k] = [tok, []]

    def _wait(self, e, deps):
        best = {}
        for sem, val in deps:
            if sem is e.sem and e.name == "pe":
                continue
            kk = id(sem)
            if val > best.get(kk, (None, 0))[1]:
                best[kk] = (sem, val)
        for kk, (sem, val) in best.items():
            if e.known.get(kk, 0) < val:
                e.h.wait_ge(sem, val)
                e.known[kk] = val

    def _split(self, kw, extra_r, extra_w):
        reads, writes = list(extra_r), list(extra_w)
        for k, v in kw.items():
            if isinstance(v, bass.AP):
                (writes if k in WRITE_KEYS else reads).append(self._key(v))
        return reads, writes

    def op(self, eng, fn, *args, R=(), W=(), **kw):
        e = self.engs[eng]
        reads, writes = self._split(kw, R, W)
        for a in args:
            if isinstance(a, bass.AP):
                reads.append(self._key(a))
        self._wait(e, self._deps(reads, writes))
        ins = getattr(e.h, fn)(*args, **kw)
        e.count += 1
        ins.then_inc(e.sem, 1)
        self._commit((e.sem, e.count), reads, writes)
        self.n_ins += 1
        return ins

    def dma(self, q, out, in_, slot, R=(), W=(), **kw):
        e = self.engs[q]
        reads = [self._key(in_)] + list(R)
        writes = [self._key(out)] + list(W)
        self._wait(e, self._deps(reads, writes))
        if slot not in self.dsem:
            self.dsem[slot] = [self.es.enter_context(self.nc.semaphore("d_" + slot)), 0]
        s = self.dsem[slot]
        e.h.dma_start(out=out, in_=in_, **kw).then_inc(s[0], 16)
        s[1] += 16
        self._commit((s[0], s[1]), reads, writes)
        self.n_ins += 1

    def barrier(self):
        toks = [(e.sem, e.count) for e in self.engs.values() if e.count]
        toks += [(s[0], s[1]) for s in self.dsem.values() if s[1]]
        for e in self.engs.values():
            self._wait(e, list(toks))
        self.reg = {}

    def finish(self):
        self.barrier()
        self.es.close()

    def mm(self, out, lhsT, rhs, start=True, stop=True):
        return self.op("pe", "matmul", out=out, lhsT=lhsT, rhs=rhs, start=start, stop=stop)

    def tr(self, out, in_, ident):
        return self.op("pe", "transpose", out=out, in_=in_, identity=ident)

    def act(self, out, in_, func, eng="act", **kw):
        return self.op(eng, "activation", out=out, in_=in_, func=func, **kw)

    def tt(self, out, in0, in1, op, eng="dve"):
        return self.op(eng, "tensor_tensor", out=out, in0=in0, in1=in1, op=op)

    def ts(self, out, in0, s1, op0, s2=None, op1=None, eng="dve", **kw):
        if op1 is None:
            return self.op(eng, "tensor_scalar", out=out, in0=in0, scalar1=s1, scalar2=None, op0=op0, **kw)
        return self.op(eng, "tensor_scalar", out=out, in0=in0, scalar1=s1, scalar2=s2, op0=op0, op1=op1, **kw)

    def stt(self, out, in0, scalar, in1, op0, op1, eng="dve", **kw):
        return self.op(eng, "scalar_tensor_tensor", out=out, in0=in0, scalar=scalar, in1=in1,
                       op0=op0, op1=op1, **kw)

    def copy(self, out, in_, eng="dve"):
        if eng == "act":
            return self.op("act", "activation", out=out, in_=in_, func=AF.Copy)
        return self.op(eng, "tensor_copy", out=out, in_=in_)

    def memset(self, ap, val, eng="pool"):
        e = self.engs[eng]
        k = self._key(ap)
        self._wait(e, self._deps([], [k]))
        ins = e.h.memset(ap, val)
        e.count += 1
        ins.then_inc(e.sem, 1)
        self._commit((e.sem, e.count), [], [k])
        self.n_ins += 1
        return ins


P_MIXR, P_MIXK, P_MIXV, P_W0, P_A0, P_KK, P_KA, P_RK, P_LNW, P_LNB = range(10)
NPAR = 10


def build(stage="rwkv", nblk=None, T=T, taps=()):
    nblk = (T // TB) if nblk is None else nblk
    nc = bass.Bass("TRN2", target_bir_lowering=False)
    b = Builder(nc)
    IN = lambda name, shape: nc.dram_tensor(name, list(shape), F32, kind="ExternalInput").ap()
    xT = IN("xT", [D, T])
    w1 = IN("w1", [D, W1C])
    gmix = IN("gmix", [128, 16])
    pu = IN("pu", [64, 2 * NPAR])
    mixl = IN("mixl", [64, 5])
    wlow = IN("wlow", [64, 5 * 128])
    kvg = IN("kvg", [128, 2])
    rw_loc = (nc.dram_tensor("rw_loc", [128, T], F32, kind="ExternalOutput").ap() if stage == "rwkv"
              else nc.dram_tensor("rw_loc", [128, T], F32).ap())
    cT_d = nc.dram_tensor("cT_d", [256, T], BF16, kind="Internal").ap()
    ikT_d = nc.dram_tensor("ikT_d", [64, T], BF16, kind="Internal").ap()

    ident = b.sb("ident", [128, 128])
    b.op("pool", "iota", ident[:], [[1, 128]], base=0, channel_multiplier=-1,
         allow_small_or_imprecise_dtypes=True, W=["ident"])
    b.ts(ident[:], ident[:], 0.0, ALU.is_equal)
    ones_b = b.sb("ones_b", [128, 128], BF16)
    b.memset(ones_b[:], 1.0)
    epst = b.sb("epst", [128, 1]); b.memset(epst[:], EPS)
    gmixt = b.sb("gmixt", [128, 16]); b.dma("sp", gmixt[:], gmix, "c0")
    b.push()
    dmat = b.sb("Wm", [64, 8, 64])
    b.op("pool", "iota", dmat[:], [[0, 8], [-1, 64]], base=0, channel_multiplier=1,
         allow_small_or_imprecise_dtypes=True, W=["Wm"])
    MS = b.sb("MS", [64, 8, 64]); MST = b.sb("MST", [64, 8, 64]); MIT = b.sb("MIT", [64, 8, 64])
    I8 = b.sb("I8", [64, 8, 64])
    b.ts(MS[:], dmat[:], 0.0, ALU.is_gt)
    b.ts(MST[:], dmat[:], 0.0, ALU.is_lt)
    b.ts(MIT[:], dmat[:], 0.0, ALU.is_le)
    b.ts(I8[:], dmat[:], 0.0, ALU.is_equal)
    ones64 = b.sb("ones64", [64, 64])
    b.memset(ones64[:], 1.0)
    mean64 = b.sb("mean64", [64, 64])
    b.memset(mean64[:], 1.0 / 64)
    segm = b.sb("segm", [64, 2 * TB])
    b.memset(segm[:], 1.0)
    for u in range(2 * TB // 64):
        b.memset(segm[:, u * 64:u * 64 + 1], 0.0)
    gnepst = b.sb("gnepst", [128, 1]); b.memset(gnepst[:], GN_EPS)

    put = b.sb("put", [64, 2, NPAR]); b.dma("sp", put[:], pu.rearrange("p (h n) -> p h n", h=2), "c0")
    mixlt = b.sb("mixlt", [64, 5]); b.dma("sp", mixlt[:], mixl, "c0")
    wlowt = b.sb("wlowt", [64, 5, 128]); b.dma("sp", wlowt[:], wlow.rearrange("p (j n) -> p j n", j=5), "c0")
    kvgt = b.sb("kvgt", [128, 2]); b.dma("sp", kvgt[:], kvg, "c0")
    mix11 = b.sb("mix11", [64, 11])
    for h in range(2):
        b.copy(mix11[:, 0 + h:1 + h], put[:, h, P_MIXR:P_MIXR + 1])
        b.copy(mix11[:, 2 + h:3 + h], put[:, h, P_MIXK:P_MIXK + 1])
        b.copy(mix11[:, 4 + h:5 + h], put[:, h, P_MIXV:P_MIXV + 1])
    b.copy(mix11[:, 6:11], mixlt[:])

    banks = [b.ps("pb%d" % i, [128, 512]) for i in range(8)]
    bank_i = [0]

    def bank():
        bank_i[0] = (bank_i[0] + 1) % 8
        return banks[bank_i[0]]

    w1b = b.sb("w1b", [128, 16, W1C], BF16)

    xs = [b.sb("xs%d" % i, [128, 16, TB]) for i in range(2)]
    for c in range(16):
        st = xs[c % 2][:].rearrange("p c t -> p (c t)")[:, 0:W1C]
        b.dma("sp", st, w1[c * 128:(c + 1) * 128, :], "x%d" % (c % 2))
        b.ts(w1b[:, c, :], st, gmixt[:, c:c + 1], ALU.mult, eng=("pool" if c % 2 else "dve"))
    xb = b.sb("xb", [128, 16, TB], BF16)
    sq = b.sb("sq", [128, 16, TB], BF16)
    rstd = b.sb("rstd", [128, TB])
    PRt = b.sb("PRt", [64, 11, TB])
    b.memset(PRt[:], 0.0)
    pcol = b.sb("pcol", [64, 11, 1]); b.memset(pcol[:], 0.0)
    Z = b.sb("Z", [64, 11, TB])
    ckv = b.sb("ckv", [128, 2, TB]); ckv2 = b.sb("ckv2", [128, 2, TB], BF16)
    cTb = b.sb("cTb", [128, 2, TB], BF16)
    ikb = b.sb("ikb", [64, TB], BF16)

    def t2(name, dt=F32):
        return b.sb(name, [64, 2, TB], dt)

    tzs = b.sb("tzs", [64, 5, TB])
    b.memset(tzs[:], 0.0)
    Aa = t2("Aa"); Gt = t2("Gt"); KK = t2("KK"); KKN = t2("KKN"); K4 = t2("K4")
    TMP = t2("TMP"); TMP2 = t2("TMP2"); BON = t2("BON"); CL = t2("CL"); LW = t2("LW")
    Pm = t2("Pm"); RpT = t2("RpT"); ApT = t2("ApT"); BmT = t2("BmT"); KmT = t2("KmT")
    BpT = t2("BpT"); KpT = t2("KpT"); KA = t2("KA")
    PL = b.sb("PL", [64, 8])
    ApM = b.sb("ApM", [64, 8, 64]); BpM = b.sb("BpM", [64, 8, 64]); KpM = b.sb("KpM", [64, 8, 64])
    VM = b.sb("VM", [64, 8, 64])
    Nb = [b.sb("Nb%d" % i, [64, 8, 64]) for i in range(2)]; Nb0 = Nb[0]
    NTb = [b.sb("NTb%d" % i, [64, 8, 64]) for i in range(2)]; NTb0 = NTb[0]
    TTm = b.sb("TTm", [64, 8, 64]); BTs = b.sb("BTs", [64, 8, 64]); CTs = b.sb("CTs", [64, 8, 64])
    ETs = b.sb("ETs", [64, 8, 64]); Wm = dmat; BVm = b.sb("BVm", [64, 8, 64])
    UVm = b.sb("UVm", [64, 8, 64]); QtT = b.sb("QtT", [64, 8, 64]); GTm = b.sb("GTm", [64, 8, 64])
    ST = b.sb("ST", [64, 2, 64]); b.memset(ST[:], 0.0)
    Yg = t2("Yg"); Yc = t2("Yc"); RWo = Yc

    GRP = [(0, 64), (64, 64), (128, 64), (192, 64), (256, 64), (320, 64),
           (384, 64), (448, 64), (512, 64), (576, 64), (640, 32)]
    xTv = xT.rearrange("(c p) t -> p c t", p=128)

    def U(h, cc):
        return slice(cc * 64, (cc + 1) * 64)

    for blk in range(nblk):
        t0 = blk * TB
        xs_ = xs[blk % 2]
        b.dma("sp", xs_[:], xTv[:, :, t0:t0 + TB], "x%d" % (blk % 2))
        b.act(sq[:], xs_[:], AF.Square)
        b.copy(xb[:], xs_[:], eng="pool")
        pss = bank()
        for c in range(16):
            b.mm(pss[:, 0:TB], ones_b[:], sq[:, c, :], start=(c == 0), stop=(c == 15))
        b.act(rstd[:], pss[:, 0:TB], AF.Sqrt, scale=1.0 / D, bias=epst[:])
        b.op("dve", "reciprocal", out=rstd[:], in_=rstd[:])
        for j, (c0, wd) in enumerate(GRP):
            pb = bank()
            for c in range(16):
                b.mm(pb[0:wd, 0:TB], w1b[:, c, c0:c0 + wd], xb[:, c, :], start=(c == 0), stop=(c == 15))
            b.tt(PRt[0:wd, j, :], pb[0:wd, 0:TB], rstd[0:wd, :], ALU.mult,
                 eng="dve")
        for j in range(2):
            pb = bank()
            for c in range(16):
                b.mm(pb[:, 0:TB], w1b[:, c, 672 + j * 128:672 + (j + 1) * 128], xb[:, c, :],
                     start=(c == 0), stop=(c == 15))
            b.tt(ckv[:, j, :], pb[:, 0:TB], rstd[:], ALU.mult)
        pb = bank()
        for c in range(16):
            b.mm(pb[0:64, 0:TB], w1b[:, c, 928:992], xb[:, c, :], start=(c == 0), stop=(c == 15))
        b.tt(ikb[:], pb[0:64, 0:TB], rstd[0:64, :], ALU.mult)
        b.dma("pool", ikT_d[:, t0:t0 + TB], ikb[:], "oik")
        b.act(ckv2[:], ckv[:], AF.Square)
        pc = bank()
        for j in range(2):
            b.mm(pc[:, 0:TB], ones_b[:], ckv2[:, j, :], start=(j == 0), stop=(j == 1))
        if blk == 0:
            build.crs = b.sb("crs", [128, TB])
        crs = build.crs
        b.act(crs[:], pc[:, 0:TB], AF.Sqrt, scale=1.0 / 256, bias=epst[:])
        b.op("dve", "reciprocal", out=crs[:], in_=crs[:])
        for j in range(2):
            b.stt(cTb[:, j, :], ckv[:, j, :], kvgt[:, j:j + 1], crs[:], ALU.mult, ALU.mult)
        b.dma("pool", cT_d.rearrange("(j p) t -> p j t", p=128)[:, :, t0:t0 + TB], cTb[:], "oc")

        b.tt(Z[:, :, 1:TB], PRt[:, :, 0:TB - 1], PRt[:, :, 1:TB], ALU.subtract)
        b.tt(Z[:, :, 0:1], pcol[:], PRt[:, :, 0:1], ALU.subtract)
        b.tt(Z[:], Z[:], mix11[:].unsqueeze(2).to_broadcast([64, 11, TB]), ALU.mult)
        b.tt(Z[:], Z[:], PRt[:], ALU.add)
        b.copy(pcol[:], PRt[:, :, TB - 1:TB], eng="pool")
        ZR = Z[:, 0:2, :]; ZK = Z[:, 2:4, :]; ZV = Z[:, 4:6, :]
        b.act(tzs[:, 0, :], Z[:, 6, :], AF.Tanh)
        b.copy(tzs[:, 1, :], Z[:, 7, :], eng="pool")
        b.act(tzs[:, 2:4, :], Z[:, 8:10, :], AF.Sigmoid)
        b.act(tzs[0:32, 4, :], Z[0:32, 10, :], AF.Sigmoid)
        for h in range(2):
            hs = slice(h * 64, (h + 1) * 64)
            pb = bank()
            b.mm(pb[0:64, 0:TB], wlowt[:, 0, hs], tzs[:, 0, :])
            b.act(LW[:, h, :], pb[0:64, 0:TB], AF.Sigmoid, bias=put[:, h, P_W0:P_W0 + 1])
            b.mm(pb[0:64, TB:2 * TB], wlowt[:, 1, hs], tzs[:, 1, :])
            b.act(Aa[:, h, :], pb[0:64, TB:2 * TB], AF.Sigmoid, bias=put[:, h, P_A0:P_A0 + 1])
            pb2 = bank()
            b.mm(pb2[0:64, 0:TB], wlowt[:, 2, hs], tzs[:, 2, :], start=True, stop=False)
            b.mm(pb2[0:64, 0:TB], wlowt[:, 3, hs], tzs[:, 3, :], start=False, stop=False)
            b.mm(pb2[0:64, 0:TB], wlowt[0:32, 4, hs], tzs[0:32, 4, :], start=False, stop=True)
            b.copy(Gt[:, h, :], pb2[0:64, 0:TB], eng="act")
            b.ts(KK[:, h, :], ZK[:, h, :], put[:, h, P_KK:P_KK + 1], ALU.mult)
            b.ts(TMP[:, h, :], Aa[:, h, :], -1.0, ALU.add, put[:, h, P_KA:P_KA + 1], ALU.mult)
            b.stt(K4[:, h, :], TMP[:, h, :], 1.0, ZK[:, h, :], ALU.add, ALU.mult)
            b.stt(TMP2[:, h, :], ZR[:, h, :], put[:, h, P_RK:P_RK + 1], K4[:, h, :], ALU.mult, ALU.mult)
        b.tt(TMP[:], KK[:], KK[:], ALU.mult)
        pn = bank()
        b.mm(pn[0:64, :], ones64[:], TMP[:].rearrange("p h t -> p (h t)"))
        b.act(KKN[:].rearrange("p h t -> p (h t)"), pn[0:64, :], AF.Sqrt)
        b.ts(KKN[:], KKN[:], 1e-12, ALU.max)
        b.op("dve", "reciprocal", out=KKN[:], in_=KKN[:])
        b.tt(KKN[:], KKN[:], KK[:], ALU.mult)
        pbn = bank()
        b.mm(pbn[0:64, :], ones64[:], TMP2[:].rearrange("p h t -> p (h t)"))
        b.tt(BON[:].rearrange("p h t -> p (h t)"), pbn[0:64, :], Z[:, 4:6, :].rearrange("p h t -> p (h t)"), ALU.mult)
        b.ts(LW[:], LW[:], -0.6065306597126334, ALU.mult)
        b.op("dve", "tensor_tensor_scan", out=CL[:].rearrange("p h t -> p (h t)"), data0=segm[:],
             data1=LW[:].rearrange("p h t -> p (h t)"), initial=0.0, op0=ALU.mult, op1=ALU.add)
        CLv = CL[:].rearrange("p h (c t) -> p (h c) t", t=64)
        b.act(Pm[:], CL[:], AF.Exp)
        b.tt(RpT[:], ZR, Pm[:], ALU.mult)
        b.copy(PL[:], Pm[:].rearrange("p h (c t) -> p (h c) t", t=64)[:, :, 63], eng="pool")
        b.tt(TMP[:], CL[:], LW[:], ALU.subtract)
        b.act(Pm[:], TMP[:], AF.Exp)
        b.stt(ApT[:], KKN[:], -1.0, Pm[:], ALU.mult, ALU.mult)
        b.act(Pm[:], CL[:], AF.Exp, scale=-1.0)
        b.tt(KA[:], KKN[:], Aa[:], ALU.mult)
        b.tt(BmT[:], KA[:], Pm[:], ALU.mult)
        b.tt(KmT[:], K4[:], Pm[:], ALU.mult)
        b.tt(TMP[:].rearrange("p h (c t) -> p (h c) t", t=64),
             CLv[:, :, 63:64].to_broadcast([64, 8, 64]), CLv, ALU.subtract)
        b.act(Pm[:], TMP[:], AF.Exp)
        b.tt(BpT[:], KA[:], Pm[:], ALU.mult)
        b.tt(KpT[:], K4[:], Pm[:], ALU.mult)

        units = [(h, cc) for h in range(2) for cc in range(4)]

        def FM(X, h, cc):
            return X[:, h, cc * 64:(cc + 1) * 64]

        for X, dst in ((ApT, ApM), (BpT, BpM), (KpT, KpM), (None, VM)):
            pb = bank()
            for u, (h, cc) in enumerate(units):
                src = FM(X, h, cc) if X is not None else Z[:, 4 + h, cc * 64:(cc + 1) * 64]
                b.tr(pb[0:64, u * 64:(u + 1) * 64], src, ident[0:64, 0:64])
            b.copy(dst[:].rearrange("p u k -> p (u k)"), pb[0:64, :], eng="act")

        def prod(L, Rr, mask, dst, eng="dve"):
            pb = bank()
            for u, (h, cc) in enumerate(units):
                b.mm(pb[0:64, u * 64:(u + 1) * 64], FM(L, h, cc), FM(Rr, h, cc))
            b.tt(dst[:].rearrange("p u k -> p (u k)"), pb[0:64, :], mask[:].rearrange("p u k -> p (u k)"),
                 ALU.mult, eng=eng)

        N, NT = Nb[0], NTb[0]
        prod(ApT, BmT, MS, N)
        prod(BmT, ApT, MST, NT)
        prod(KmT, ApT, MST, BTs)
        prod(BmT, RpT, MIT, CTs)
        prod(KmT, RpT, MIT, ETs)
        b.tt(TTm[:], NT[:], I8[:], ALU.add)
        for i in range(1, 6):
            N2, NT2 = Nb[i % 2], NTb[i % 2]
            pb = bank()
            for u in range(8):
                b.mm(pb[0:64, u * 64:(u + 1) * 64], NT[:, u, :], N[:, u, :])
            b.copy(N2[:].rearrange("p u k -> p (u k)"), pb[0:64, :], eng="act")
            if i < 5:
                pb2 = bank()
                for u in range(8):
                    b.mm(pb2[0:64, u * 64:(u + 1) * 64], N[:, u, :], NT[:, u, :])
                b.copy(NT2[:].rearrange("p u k -> p (u k)"), pb2[0:64, :])
            pb3 = bank()
            for u in range(8):
                b.mm(pb3[0:64, u * 64:(u + 1) * 64], N2[:, u, :], TTm[:, u, :])
            b.tt(TTm[:].rearrange("p u k -> p (u k)"), TTm[:].rearrange("p u k -> p (u k)"), pb3[0:64, :], ALU.add)
            N, NT = N2, NT2

        def prod2(Lm, Rm, dst, eng="act"):
            pb = bank()
            for u in range(8):
                b.mm(pb[0:64, u * 64:(u + 1) * 64], Lm[:, u, :], Rm[:, u, :])
            if dst is not None:
                b.copy(dst[:].rearrange("p u k -> p (u k)"), pb[0:64, :], eng=eng)
            return pb

        prod2(TTm, ApM, Wm)
        prod2(BTs, VM, BVm, eng="dve")
        prod2(TTm, BVm, UVm)
        pq = prod2(Wm, CTs, None)
        b.tt(QtT[:].rearrange("p (h c) t -> p h (c t)", h=2), pq[0:64, :].rearrange("p (h x) -> p h x", h=2),
             RpT[:], ALU.add)
        prod2(Wm, BpM, GTm, eng="dve")
        pY = bank(); pS = bank()
        for cc in range(4):
            for h in range(2):
                u = h * 4 + cc
                us = slice(u * 64, (u + 1) * 64)
                b.mm(pY[0:64, us], UVm[:, u, :], CTs[:, u, :], start=True, stop=False)
                b.mm(pY[0:64, us], VM[:, u, :], ETs[:, u, :], start=False, stop=False)
                b.mm(pY[0:64, us], ST[:, h, :], QtT[:, u, :], start=False, stop=True)
                b.mm(pS[0:64, us], BpM[:, u, :], UVm[:, u, :], start=True, stop=False)
                b.mm(pS[0:64, us], KpM[:, u, :], VM[:, u, :], start=False, stop=False)
                b.mm(pS[0:64, us], GTm[:, u, :], ST[:, h, :], start=False, stop=True)
            for h in range(2):
                u = h * 4 + cc
                us = slice(u * 64, (u + 1) * 64)
                b.stt(ST[:, h, :], ST[:, h, :], PL[:, u:u + 1], pS[0:64, us], ALU.mult, ALU.add)
        Ygf = Yg[:].rearrange("p h t -> p (h t)")
        b.copy(Ygf, pY[0:64, :], eng="act")
        pm = bank()
        b.mm(pm[0:64, :], mean64[:], Ygf)
        Ycf = Yc[:].rearrange("p h t -> p (h t)")
        b.tt(Ycf, Ygf, pm[0:64, :], ALU.subtract)
        b.tt(TMP[:], Yc[:], Yc[:], ALU.mult)
        pv = bank()
        b.mm(pv[0:64, :], mean64[:], TMP[:].rearrange("p h t -> p (h t)"))
        b.act(TMP2[:].rearrange("p h t -> p (h t)"), pv[0:64, :], AF.Sqrt, bias=gnepst[0:64, :])
        b.op("dve", "reciprocal", out=TMP2[:], in_=TMP2[:])
        b.tt(Yc[:], Yc[:], TMP2[:], ALU.mult)
        for h in range(2):
            b.ts(Yc[:, h, :], Yc[:, h, :], put[:, h, P_LNW:P_LNW + 1], ALU.mult,
                 put[:, h, P_LNB:P_LNB + 1], ALU.add)
        b.tt(Yc[:], Yc[:], BON[:], ALU.add)
        b.tt(RWo[:], Yc[:], Gt[:], ALU.mult)
        b.dma("pool", rw_loc.rearrange("(h p) t -> p h t", p=64)[:, :, t0:t0 + TB], RWo[:], "orw")
        if blk == 0 and taps:
            loc = dict(locals())
            for nm in taps:
                tl = loc[nm]
                shp = list(tl.shape)
                o = nc.dram_tensor("tap_" + nm, shp, F32, kind="ExternalOutput").ap()
                b.dma("pool", o, tl[:], "otap")

    b.pop()
    if stage == "rwkv":
        b.finish()
        return nc, b

    NS = T // 1024
    NTOK = NS * 128
    NKT = T // 128
    NCH = min(512, NTOK)
    DT = lambda name, shape, dt=F32: nc.dram_tensor(name, list(shape), dt).ap()

    sel = IN("sel", [128, 8])
    rw_all = DT("rw_all", [1024, T])
    rwo_d = DT("rwo_d", [1024, NTOK], BF16)
    e = b.engs["pool"]
    b._wait(e, b._deps(["rw_loc"], ["rw_all"]))
    csem = b.es.enter_context(nc.semaphore("csem"))
    nc.gpsimd.collective_compute("AllGather", ALU.bypass, replica_groups=[list(range(NCORES))],
                                 ins=[rw_loc.opt()], outs=[rw_all.opt()]).then_inc(csem)
    b._commit((csem, 1), ["rw_loc"], ["rw_all"])
    b.push()
    selt = b.sb("selt", [128, 8]); b.dma("sp", selt[:], sel, "c0")
    gblk = [b.sb("gblk%d" % i, [128, 8, 1024]) for i in range(2)]
    rwo = b.sb("rwo", [128, 8, NS, 128])
    rwob = b.sb("rwob", [128, 8, NS, 128], BF16)
    rav = rw_all.rearrange("(c p) t -> p c t", p=128)
    for m in range(NS):
        g = gblk[m % 2]
        b.dma("pool", g[:], rav[:, :, m * 1024:(m + 1) * 1024], "g%d" % (m % 2))
        gv = g[:].rearrange("p c (j i) -> p c j i", j=8)
        b.ts(rwo[:, :, m, :], gv[:, :, 0, :], selt[:, 0:1], ALU.mult)
        for j in range(1, 8):
            b.stt(rwo[:, :, m, :], gv[:, :, j, :], selt[:, j:j + 1], rwo[:, :, m, :], ALU.mult, ALU.add)
    b.copy(rwob[:], rwo[:], eng="act")
    b.dma("sp", rwo_d.rearrange("(c p) t -> p c t", p=128), rwob[:].rearrange("p c m i -> p c (m i)"), "og")
    b.pop()

    xTo = IN("xTo", [D, NTOK])
    w3q = IN("w3q", [D, 1024]); w3iq = IN("w3iq", [D, 512]); w3iw = IN("w3iw", [D, 8])
    q_d = DT("q_d", [64, 16 * NTOK], BF16)
    xbo_d = DT("xbo_d", [D, NTOK], BF16)
    rso_d = DT("rso_d", [128, NTOK])
    iq_d = DT("iq_d", [64, 8 * NTOK], BF16)
    identb = b.sb("identb", [128, 128], BF16)
    b.copy(identb[:], ident[:])
    iwTM = b.sb("iwTM", [128, NS, 8])
    xov = xTo.rearrange("(c p) t -> p c t", p=128)
    b.push()
    xbo = b.sb("xbo", [128, 16, NTOK], BF16)
    sqo = b.sb("sqo", [128, 16, NTOK], BF16)
    rso = b.sb("rso", [128, NTOK])
    b.push()
    xo = b.sb("xo", [128, 16, NTOK])
    b.dma("sp", xo[:], xov, "x0")
    b.act(sqo[:], xo[:], AF.Square)
    for c in range(16):
        b.ts(xbo[:, c, :], xo[:, c, :], gmixt[:, c:c + 1], ALU.mult, eng=("pool" if c % 2 else "dve"))
    b.pop()
    qTs = b.sb("qTs", [64, 16, NTOK], BF16)
    iqTs = b.sb("iqTs", [64, 8, NTOK], BF16)
    iwTs = b.sb("iwTs", [8, NTOK])
    wst3 = [b.sb("wst3_%d" % i, [128, 16, 64]) for i in range(2)]
    wb3 = [b.sb("wb3_%d" % i, [128, 16, 64], BF16) for i in range(2)]
    for n0 in range(0, NTOK, NCH):
        pss = bank()
        for c in range(16):
            b.mm(pss[:, 0:NCH], ones_b[:], sqo[:, c, n0:n0 + NCH], start=(c == 0), stop=(c == 15))
        b.act(rso[:, n0:n0 + NCH], pss[:, 0:NCH], AF.Sqrt, scale=1.0 / D, bias=epst[:])
    b.op("dve", "reciprocal", out=rso[:], in_=rso[:])
    groups = [(w3q, h * 64, 64, qTs, h) for h in range(16)] + [(w3iq, h * 64, 64, iqTs, h) for h in range(8)] \
        + [(w3iw, 0, 8, iwTs, None)]
    for gi, (wsrc, c0, wd, dst, hh) in enumerate(groups):
        st = wst3[gi % 2]; wb = wb3[gi % 2]
        b.dma("sp", st[:, :, 0:wd], wsrc.rearrange("(c p) n -> p c n", p=128)[:, :, c0:c0 + wd], "w3_%d" % (gi % 2))
        b.copy(wb[:, :, 0:wd], st[:, :, 0:wd], eng="act")
        for n0 in range(0, NTOK, NCH):
            pb = bank()
            for c in range(16):
                b.mm(pb[0:wd, 0:NCH], wb[:, c, 0:wd], xbo[:, c, n0:n0 + NCH], start=(c == 0), stop=(c == 15))
            o = dst[:, hh, n0:n0 + NCH] if hh is not None else dst[:, n0:n0 + NCH]
            b.tt(o, pb[0:wd, 0:NCH], rso[0:wd, n0:n0 + NCH], ALU.mult)
    b.dma("sp", xbo_d.rearrange("(c p) t -> p c t", p=128), xbo[:], "oq")
    b.dma("sp", rso_d, rso[:], "oq")
    b.dma("sp", q_d.rearrange("p (h t) -> p h t", h=16), qTs[:], "oq")
    b.dma("sp", iq_d.rearrange("p (h t) -> p h t", h=8), iqTs[:], "oq")
    for m in range(NS):
        pb = bank()
        b.tr(pb[:, 0:8], iwTs[:, m * 128:(m + 1) * 128], ident[0:8, 0:8])
        b.ts(iwTM[:, m, :], pb[:, 0:8], IDX_SCALE, ALU.mult)
    b.pop()

    capi = IN("cap", [128, 1024])
    ohc = IN("ohc", [32, NVEC])
    relb = IN("relb", [32, 16]); relb15 = IN("relb15", [16, 1])
    wukT = IN("wukT", [64, 16 * 256]); wuvp = IN("wuvp", [128, 16 * 2 * 128])
    vec_d = DT("vec_d", [16, NVEC])
    EBn_d = DT("EBn_d", [16 * 128, 2048], BF16)
    dsa_d = DT("dsa_d", [1024, NTOK], BF16)
    b.push()
    cT = b.sb("cT", [128, 2, T], BF16)
    ikT = b.sb("ikT", [64, T], BF16)
    cTM = b.sb("cTM", [128, NKT, 256], BF16)
    sc = b.sb("sc", [128, T])
    msel = b.sb("msel", [128, T], BF16)
    MT = sc[:].bitcast(BF16)[:, 0:NKT * 128].rearrange("p (k q) -> p k q", q=128)
    capt = b.sb("capt", [128, 1024])
    wukb = b.sb("wukb", [64, 16, 256], BF16)
    wuvb = b.sb("wuvb", [128, 16, 2, 128], BF16)
    dsaTs = [b.sb("dsaTs%d" % i, [128, 8, 128], BF16) for i in range(2)]
    Jm = b.sb("Jm", [128, 128])
    b.op("pool", "iota", Jm[:], [[1, 128]], base=-127, channel_multiplier=1,
         allow_small_or_imprecise_dtypes=True, W=["Jm"])
    b.ts(Jm[:], Jm[:], 0.0, ALU.is_equal)
    b.dma("sp", cT[:], cT_d.rearrange("(j p) t -> p j t", p=128), "x0")
    b.dma("sp", ikT[:], ikT_d, "x1")
    b.dma("sp", capt[:], capi, "c0")
    b.push()
    scw = sc[:, 0:4096] if T >= 4096 else b.sb("scw", [128, 4096])[:, :]
    b.dma("sp", scw[0:64, :], wukT, "w3_0")
    b.copy(wukb[:].rearrange("p h r -> p (h r)"), scw[0:64, :], eng="act")
    b.dma("sp", scw, wuvp, "w3_1")
    b.copy(wuvb[:].rearrange("p h c n -> p (h c n)"), scw, eng="act")
    b.pop()
    pbf = lambda pb: pb[:].bitcast(BF16)
    for k0 in range(0, NKT, 4):
        pb = bank()
        v = pbf(pb)
        for k in range(4):
            for rc in range(2):
                b.tr(v[:, (k * 2 + rc) * 128:(k * 2 + rc + 1) * 128], cT[:, rc, (k0 + k) * 128:(k0 + k + 1) * 128], identb[:])
        b.copy(cTM[:, k0:k0 + 4, :].rearrange("p k r -> p (k r)"), v[:, 0:1024], eng="act")
    b.push()
    relbt = b.sb("relbt", [32, 16]); b.dma("sp", relbt[:], relb, "c0")
    nb15 = b.sb("nb15", [16, 1]); b.dma("sp", nb15[:], relb15, "c0")
    b.ts(nb15[:], nb15[:], -1.0, ALU.mult)
    oht = b.sb("oht", [32, NVEC]); b.dma("sp", oht[:], ohc, "c0")
    vecs = b.sb("vecs", [16, NVEC])
    for x0 in range(0, NVEC, 512):
        xw = min(512, NVEC - x0)
        pb = bank()
        b.mm(pb[0:16, 0:xw], relbt[:], oht[:, x0:x0 + xw])
        b.act(vecs[:, x0:x0 + xw], pb[0:16, 0:xw], AF.Exp, bias=nb15[:])
    b.dma("sp", vec_d, vecs[:], "ov")
    Tp = [b.sb("Tp%d" % i, [128, 16, 128]) for i in range(2)]
    EBs = [b.sb("EBs%d" % i, [128, 16, 128], BF16) for i in range(2)]
    for jj in range(16):
        tp = Tp[jj % 2]; ebs = EBs[jj % 2]
        hank = bass.AP(vec_d.tensor, 128 * jj, [[1, 128], [NVEC, 16], [1, 128]])
        b.dma("sp", tp[:], hank, "hk%d" % (jj % 2), R=["vec_d"])
        for h4 in range(4):
            pb = bank()
            for k in range(4):
                b.mm(pb[:, k * 128:(k + 1) * 128], tp[:, h4 * 4 + k, :], Jm[:])
            b.copy(ebs[:, h4 * 4:h4 * 4 + 4, :].rearrange("p h q -> p (h q)"), pb[:], eng="act")
        b.dma("sp", EBn_d[jj * 128:(jj + 1) * 128, :], ebs[:].rearrange("p h q -> p (h q)"), "oeb%d" % (jj % 2))
    b.pop()

    qs = b.sb("qs", [64, 16, 128], BF16); iqs = b.sb("iqs", [64, 8, 128], BF16)
    qlT = b.sb("qlT", [128, 2, 2048], BF16); olT = b.sb("olT", [128, 2, 2048], BF16)
    tmpS = [b.sb("tmpS%d" % i, [128, 512]) for i in range(2)]
    Et = [b.sb("Et%d" % i, [128, 4, 128], BF16) for i in range(2)]
    EBt = [b.sb("EBt%d" % i, [128, 4, 128], BF16) for i in range(2)]
    rden = b.sb("rden", [128, 512])
    lo = b.sb("lo", [128, 1]); mid = b.sb("mid", [128, 1]); cnt = b.sb("cnt", [128, 1]); gg = b.sb("gg", [128, 1])
    q_dv = q_d.rearrange("p (h t) -> p h t", h=16); iq_dv = iq_d.rearrange("p (h t) -> p h t", h=8)
    EBn_v = EBn_d.rearrange("(j s) (h q) -> j s h q", s=128, h=16)
    bank5 = [0]

    def bank_lo():
        bank5[0] = (bank5[0] + 1) % 5
        return banks[bank5[0]]

    po0, po1, pdn = banks[5], banks[6], banks[7]
    for m in range(NS):
        nkt = 8 * m + 8
        S = nkt * 128
        tsl = slice(m * 128, (m + 1) * 128)
        b.dma("sp", qs[:], q_dv[:, :, tsl], "qs")
        b.dma("sp", iqs[:], iq_dv[:, :, tsl], "qs")
        for sk in range(S // 512):
            ssl = slice(sk * 512, (sk + 1) * 512)
            for h in range(8):
                pb = bank_lo()
                b.mm(pb[:, :], iqs[:, h, :], ikT[:, ssl])
                if h == 0:
                    b.ts(sc[:, ssl], pb[:, :], 0.0, ALU.max, iwTM[:, m, 0:1], ALU.mult)
                else:
                    tm = tmpS[h % 2]
                    b.ts(tm[:], pb[:, :], 0.0, ALU.max, iwTM[:, m, h:h + 1], ALU.mult)
                    b.tt(sc[:, ssl], sc[:, ssl], tm[:], ALU.add, eng="pool")
        b.tt(sc[:, S - 1024:S], sc[:, S - 1024:S], capt[:], ALU.min)
        b.memset(lo[:], -1024.0, eng="dve")
        for k in range(33):
            hk = 1024.0 / (2 ** k)
            b.ts(mid[:], lo[:], hk, ALU.add)
            b.op("dve", "tensor_scalar", out=msel[:, 0:S], in0=sc[:, 0:S], scalar1=mid[:, 0:1], scalar2=None,
                 op0=ALU.is_ge, op1=ALU.add, accum_out=cnt[:])
            b.ts(gg[:], cnt[:], 255.5, ALU.is_ge, hk, ALU.mult)
            b.tt(lo[:], lo[:], gg[:], ALU.add)
        b.ts(msel[:, 0:S], sc[:, 0:S], lo[:, 0:1], ALU.is_ge)
        for k0 in range(0, nkt, 8):
            pb = bank_lo()
            v = pbf(pb)
            for k in range(8):
                b.tr(v[:, k * 128:(k + 1) * 128], msel[:, (k0 + k) * 128:(k0 + k + 1) * 128], identb[:])
            b.copy(MT[:, k0:k0 + 8, :], v[:, 0:1024].rearrange("p (k q) -> p k q", q=128), eng="act")
        for rc in range(2):
            for h4 in range(4):
                pb = bank_lo()
                for k in range(4):
                    h = h4 * 4 + k
                    b.mm(pb[:, k * 128:(k + 1) * 128], wukb[:, h, rc * 128:(rc + 1) * 128], qs[:, h, :])
                b.copy(qlT[:, rc, h4 * 512:(h4 + 1) * 512], pb[:, :], eng="act")
        for p in range(4):
            psl = slice(p * 512, (p + 1) * 512)
            for kt in range(nkt):
                ksl = slice(kt * 128, (kt + 1) * 128)
                pl = bank_lo()
                b.mm(pl[:, :], cT[:, 0, ksl], qlT[:, 0, psl], start=True, stop=False)
                b.mm(pl[:, :], cT[:, 1, ksl], qlT[:, 1, psl], start=False, stop=True)
                E = Et[kt % 2]
                Ef = E[:].rearrange("p h q -> p (h q)")
                b.act(Ef, pl[:, :], AF.Exp, scale=SA_SCALE)
                b.tt(E[:], E[:], MT[:, kt, :].unsqueeze(1).to_broadcast([128, 4, 128]), ALU.mult)
                jj = kt - (8 * m - 8)
                if jj >= 0:
                    ebt = EBt[kt % 2]
                    b.dma("sp", ebt[:], EBn_v[jj, :, p * 4:(p + 1) * 4, :], "eb%d" % (kt % 2))
                    b.tt(E[:], E[:], ebt[:], ALU.mult, eng="pool")
                st, sp_ = (kt == 0), (kt == nkt - 1)
                b.mm(po0[:, :], cTM[:, kt, 0:128], Ef, start=st, stop=sp_)
                b.mm(po1[:, :], cTM[:, kt, 128:256], Ef, start=st, stop=sp_)
                b.mm(pdn[:, :], ones_b[:], Ef, start=st, stop=sp_)
            b.op("dve", "reciprocal", out=rden[:], in_=pdn[:, :])
            b.tt(olT[:, 0, psl], po0[:, :], rden[:], ALU.mult)
            b.tt(olT[:, 1, psl], po1[:, :], rden[:], ALU.mult)
        for hc in range(8):
            pb = bank_lo()
            i = 0
            for h in (2 * hc, 2 * hc + 1):
                for rc in range(2):
                    b.mm(pb[:, 0:128], wuvb[:, h, rc, :], olT[:, rc, h * 128:(h + 1) * 128], start=(i == 0), stop=(i == 3))
                    i += 1
            b.copy(dsaTs[m % 2][:, hc, :], pb[:, 0:128], eng="act")
        b.dma("sp", dsa_d.rearrange("(c p) t -> p c t", p=128)[:, :, tsl], dsaTs[m % 2][:], "od%d" % (m % 2))
    b.pop()
    if stage == "dsa":
        o = nc.dram_tensor("dsa_out", [128, 8 * NTOK], BF16, kind="ExternalOutput").ap()
        b.dma("sp", o.rearrange("p (c t) -> p c t", c=8), dsa_d.rearrange("(c p) t -> p c t", p=128), "odbg")
    if stage == "dsa":
        b.finish()
        return nc, b

    w3g = IN("w3g", [D, 4096]); wbr = IN("wbr", [1024, D]); wbd = IN("wbd", [1024, D]); wout = IN("wout", [D, D])
    h1_d = DT("h1_d", [D, NTOK])
    b.push()
    xbo = b.sb("xbo2", [128, 16, NTOK], BF16)
    rso = b.sb("rso2", [128, NTOK])
    rwT = b.sb("rwT", [128, 8, NTOK], BF16)
    dsT = b.sb("dsT", [128, 8, NTOK], BF16)
    mT = b.sb("mT", [128, 16, NTOK], BF16)
    wsg = [b.sb("wsg%d" % i, [128, 16, 128]) for i in range(2)]
    wbg = [b.sb("wbg%d" % i, [128, 16, 128], BF16) for i in range(2)]
    gA = b.sb("gA", [128, NTOK]); gB = b.sb("gB", [128, NTOK]); mtmp = b.sb("mtmp", [128, NTOK])
    xres = [b.sb("xres%d" % i, [128, NTOK]) for i in range(2)]
    hch = [b.sb("hch%d" % i, [128, NTOK]) for i in range(2)]
    b.dma("sp", xbo[:], xbo_d.rearrange("(c p) t -> p c t", p=128), "x0")
    b.dma("sp", rso[:], rso_d, "x1")
    b.dma("sp", rwT[:], rwo_d.rearrange("(c p) t -> p c t", p=128), "x0")
    b.dma("sp", dsT[:], dsa_d.rearrange("(c p) t -> p c t", p=128), "x1")
    wcnt = [0]

    def load_w(view, kc):
        i = wcnt[0] % 2
        wcnt[0] += 1
        b.dma("sp", wsg[i][:, 0:kc, :], view, "wg%d" % i)
        b.copy(wbg[i][:, 0:kc, :], wsg[i][:, 0:kc, :], eng=("act" if i else "pool"))
        return wbg[i]

    w3gv = w3g.rearrange("(c p) n -> p c n", p=128)
    wbrv = wbr.rearrange("(c p) n -> p c n", p=128)
    wbdv = wbd.rearrange("(c p) n -> p c n", p=128)
    woutv = wout.rearrange("(c p) n -> p c n", p=128)
    for dc in range(16):
        cs = slice(dc * 128, (dc + 1) * 128)
        for which, gt in ((0, gA), (1, gB)):
            wb = load_w(w3gv[:, :, which * 2048 + dc * 128: which * 2048 + (dc + 1) * 128], 16)
            for n0 in range(0, NTOK, NCH):
                pb = bank()
                for c in range(16):
                    b.mm(pb[:, 0:NCH], wb[:, c, :], xbo[:, c, n0:n0 + NCH], start=(c == 0), stop=(c == 15))
                b.tt(gt[:, n0:n0 + NCH], pb[:, 0:NCH], rso[:, n0:n0 + NCH], ALU.mult)
            b.act(gt[:], gt[:], AF.Sigmoid)
        wb = load_w(wbrv[:, :, cs], 8)
        for n0 in range(0, NTOK, NCH):
            pb = bank()
            for c in range(8):
                b.mm(pb[:, 0:NCH], wb[:, c, :], rwT[:, c, n0:n0 + NCH], start=(c == 0), stop=(c == 7))
            b.tt(mtmp[:, n0:n0 + NCH], pb[:, 0:NCH], gA[:, n0:n0 + NCH], ALU.mult)
        wb = load_w(wbdv[:, :, cs], 8)
        for n0 in range(0, NTOK, NCH):
            pb = bank()
            for c in range(8):
                b.mm(pb[:, 0:NCH], wb[:, c, :], dsT[:, c, n0:n0 + NCH], start=(c == 0), stop=(c == 7))
            b.tt(gB[:, n0:n0 + NCH], pb[:, 0:NCH], gB[:, n0:n0 + NCH], ALU.mult)
        b.tt(mT[:, dc, :], mtmp[:], gB[:], ALU.add)
    for dc in range(16):
        cs = slice(dc * 128, (dc + 1) * 128)
        wb = load_w(woutv[:, :, cs], 16)
        xr = xres[dc % 2]; hc_ = hch[dc % 2]
        b.dma("sp", xr[:], xTo[cs, :], "xr%d" % (dc % 2))
        for n0 in range(0, NTOK, NCH):
            pb = bank()
            for c in range(16):
                b.mm(pb[:, 0:NCH], wb[:, c, :], mT[:, c, n0:n0 + NCH], start=(c == 0), stop=(c == 15))
            b.tt(hc_[:, n0:n0 + NCH], pb[:, 0:NCH], xr[:, n0:n0 + NCH], ALU.add)
        b.dma("sp", h1_d[cs, :], hc_[:], "oh%d" % (dc % 2))
    if stage in ("mix", "full_dbg"):
        o = nc.dram_tensor("h1_out", [D, NTOK], F32, kind="ExternalOutput").ap()
        b.dma("sp", o, h1_d, "odbg2")
    b.pop()
    if stage == "mix":
        b.finish()
        return nc, b

    gffn = IN("gffn", [128, 16]); wpq = IN("wpq", [D, D]); skT = IN("skT", [128, 16 * 128])
    puT = IN("puT", [D, 16384]); pv = IN("pv", [16384, D]); gfin = IN("gfin", [1, D])
    out_own = nc.dram_tensor("out_own", [NTOK, D], F32, kind="ExternalOutput").ap()
    PH = min(512, NTOK)
    NTL = PH // 128
    gffnt = b.sb("gffnt", [128, 16]); b.dma("sp", gffnt[:], gffn, "c0")
    skTt = b.sb("skTt", [128, 16, 128]); b.dma("sp", skTt[:], skT.rearrange("p (g n) -> p g n", g=16), "c0")
    gfbc = b.sb("gfbc", [128, D])
    b.dma("sp", gfbc[:], bass.AP(gfin.tensor, 0, [[0, 128], [1, D]]), "c0")
    wpqv = wpq.rearrange("(c p) n -> p c n", p=128)
    puTv = puT.rearrange("(c p) e -> p c e", p=128)
    for ps_ in range(NTOK // PH):
        t0 = ps_ * PH
        b.push()
        hnT = b.sb("hnT", [128, 16, PH], BF16)
        acc = b.sb("acc", [128, NTL, D])
        S12 = b.sb("S12", [128, NTL, 16, 128])
        tau = b.sb("tau", [128, NTL, 8]); negm = b.sb("negm", [128, NTL, 8])
        b.push()
        h1p = b.sb("h1p", [128, 16, PH])
        rs2 = b.sb("rs2", [128, PH])
        q2T = b.sb("q2T", [128, 16, PH])
        sqp = q2T[:].rearrange("p g t -> p (g t)").bitcast(BF16)[:, 0:16 * PH].rearrange("p (g t) -> p g t", g=16)
        wsq = [b.sb("wsq%d" % i, [128, 16, 128]) for i in range(2)]
        wbq = [b.sb("wbq%d" % i, [128, 16, 128], BF16) for i in range(2)]
        b.dma("sp", h1p[:], h1_d.rearrange("(c p) t -> p c t", p=128)[:, :, t0:t0 + PH], "x0")
        b.act(sqp, h1p[:], AF.Square)
        pss = bank()
        for c in range(16):
            b.mm(pss[:, 0:PH], ones_b[:], sqp[:, c, :], start=(c == 0), stop=(c == 15))
        b.act(rs2[:], pss[:, 0:PH], AF.Sqrt, scale=1.0 / D, bias=epst[:])
        b.op("dve", "reciprocal", out=rs2[:], in_=rs2[:])
        for c in range(16):
            b.stt(hnT[:, c, :], h1p[:, c, :], gffnt[:, c:c + 1], rs2[:], ALU.mult, ALU.mult)
        for tl in range(NTL):
            for d4 in range(4):
                pb = bank()
                for k in range(4):
                    b.tr(pb[:, k * 128:(k + 1) * 128], h1p[:, d4 * 4 + k, tl * 128:(tl + 1) * 128], ident[:])
                b.copy(acc[:, tl, d4 * 512:(d4 + 1) * 512], pb[:, :], eng="act")
        for g in range(16):
            i = g % 2
            b.dma("sp", wsq[i][:], wpqv[:, :, g * 128:(g + 1) * 128], "wq%d" % i)
            b.copy(wbq[i][:], wsq[i][:], eng=("act" if i else "pool"))
            pb = bank()
            for c in range(16):
                b.mm(pb[:, 0:PH], wbq[i][:, c, :], hnT[:, c, :], start=(c == 0), stop=(c == 15))
            b.copy(q2T[:, g, :], pb[:, 0:PH])
        for tl in range(NTL):
            for g4 in range(4):
                pb = bank()
                for k in range(4):
                    g = g4 * 4 + k
                    b.mm(pb[:, k * 128:(k + 1) * 128], q2T[:, g, tl * 128:(tl + 1) * 128], skTt[:, g, :])
                b.copy(S12[:, tl, g4 * 4:g4 * 4 + 4, :].rearrange("p g n -> p (g n)"), pb[:, :], eng="act")
        b.pop()
        b.push()
        v12 = b.sb("v12", [128, 2, 16]); mrt = b.sb("mrt", [128, 256]); cand = b.sb("cand", [128, 16, 16])
        c16 = b.sb("c16", [128, 16]); e16 = b.sb("e16", [128, 16]); nmx = b.sb("nmx", [128, 1]); zz = b.sb("zz", [128, 1])
        for tl in range(NTL):
            for h in range(8):
                for cc in range(2):
                    src = S12[:, tl, 2 * h + cc, :]
                    b.op("dve", "max", out=v12[:, cc, 0:8], in_=src)
                    b.op("dve", "match_replace", out=mrt[:, 0:128], in_to_replace=v12[:, cc, 0:8], in_values=src, imm_value=-1e30)
                    b.op("dve", "max", out=v12[:, cc, 8:16], in_=mrt[:, 0:128])
                b.tt(cand[:], v12[:, 0, :].unsqueeze(2).to_broadcast([128, 16, 16]),
                     v12[:, 1, :].unsqueeze(1).to_broadcast([128, 16, 16]), ALU.add)
                cf = cand[:].rearrange("p a b -> p (a b)")
                b.op("dve", "max", out=c16[:, 0:8], in_=cf)
                b.op("dve", "match_replace", out=mrt[:], in_to_replace=c16[:, 0:8], in_values=cf, imm_value=-1e30)
                b.op("dve", "max", out=c16[:, 8:16], in_=mrt[:])
                b.copy(tau[:, tl, h:h + 1], c16[:, 15:16])
                b.ts(nmx[:], c16[:, 0:1], -1.0, ALU.mult)
                b.act(e16[:], c16[:], AF.Exp, bias=nmx[:])
                b.op("dve", "reduce_sum", out=zz[:], in_=e16[:], axis=AX.X)
                b.act(zz[:], zz[:], AF.Ln)
                b.tt(negm[:, tl, h:h + 1], nmx[:], zz[:], ALU.subtract)
        b.pop()
        b.push()
        ust = [b.sb("ust%d" % i, [128, 4, 512]) for i in range(2)]
        uTb = b.sb("uTb", [128, 16, 512], BF16)
        vst = [b.sb("vst%d" % i, [128, D]) for i in range(2)]
        vb = b.sb("vb", [128, 4, D], BF16)
        GE = b.sb("GE", [128, 512]); Xt = [b.sb("Xt%d" % i, [128, 4, 128]) for i in range(2)]
        Ee = [b.sb("Ee%d" % i, [128, 4, 128]) for i in range(2)]
        Gs = b.sb("Gs", [128, 4, 128]); Gh = [b.sb("Gh%d" % i, [128, 4, 128]) for i in range(2)]
        GA = b.sb("GA", [128, 512], BF16); GAT = b.sb("GAT", [128, 4, 128], BF16)
        for eb in range(32):
            i0 = eb * 4
            for q4 in range(4):
                st = ust[q4 % 2]
                b.dma("sp", st[:], puTv[:, q4 * 4:(q4 + 1) * 4, eb * 512:(eb + 1) * 512], "us%d" % (q4 % 2))
                b.copy(uTb[:, q4 * 4:(q4 + 1) * 4, :], st[:], eng=("act" if q4 % 2 else "pool"))
            for ec in range(4):
                st = vst[ec % 2]
                b.dma("sp", st[:], pv[eb * 512 + ec * 128: eb * 512 + (ec + 1) * 128, :], "vs%d" % (ec % 2))
                b.copy(vb[:, ec, :], st[:], eng=("pool" if ec % 2 else "act"))
            for tl in range(NTL):
                tsl = slice(tl * 128, (tl + 1) * 128)
                pa = bank()
                for c in range(16):
                    b.mm(pa[:, :], hnT[:, c, tsl], uTb[:, c, :], start=(c == 0), stop=(c == 15))
                b.act(GE[:], pa[:, :], AF.Gelu)
                for h in range(8):
                    X = Xt[h % 2]; E = Ee[h % 2]
                    b.tt(X[:], S12[:, tl, 2 * h, i0:i0 + 4].unsqueeze(2).to_broadcast([128, 4, 128]),
                         S12[:, tl, 2 * h + 1, :].unsqueeze(1).to_broadcast([128, 4, 128]), ALU.add)
                    b.act(E[:], X[:], AF.Exp, bias=negm[:, tl, h:h + 1])
                    if h == 0:
                        b.stt(Gs[:], X[:], tau[:, tl, h:h + 1], E[:], ALU.is_ge, ALU.mult)
                    else:
                        g_ = Gh[h % 2]
                        b.stt(g_[:], X[:], tau[:, tl, h:h + 1], E[:], ALU.is_ge, ALU.mult)
                        b.tt(Gs[:], Gs[:], g_[:], ALU.add, eng="pool")
                b.tt(GA[:], Gs[:].rearrange("p a j -> p (a j)"), GE[:], ALU.mult)
                pb = bank()
                v = pbf(pb)
                for ec in range(4):
                    b.tr(v[:, ec * 128:(ec + 1) * 128], GA[:, ec * 128:(ec + 1) * 128], identb[:])
                b.copy(GAT[:].rearrange("p a t -> p (a t)"), v[:, 0:512], eng="act")
                for d4 in range(4):
                    pb = bank()
                    for ec in range(4):
                        b.mm(pb[:, :], GAT[:, ec, :], vb[:, ec, d4 * 512:(d4 + 1) * 512], start=(ec == 0), stop=(ec == 3))
                    b.tt(acc[:, tl, d4 * 512:(d4 + 1) * 512], acc[:, tl, d4 * 512:(d4 + 1) * 512], pb[:, :], ALU.add)
        b.pop()
        b.push()
        sqf = b.sb("sqf", [128, D]); ssf = b.sb("ssf", [128, 1]); of_ = [b.sb("of%d" % i, [128, D]) for i in range(2)]
        for tl in range(NTL):
            b.tt(sqf[:], acc[:, tl, :], acc[:, tl, :], ALU.mult)
            b.op("dve", "reduce_sum", out=ssf[:], in_=sqf[:], axis=AX.X)
            b.act(ssf[:], ssf[:], AF.Sqrt, scale=1.0 / D, bias=epst[:])
            b.op("dve", "reciprocal", out=ssf[:], in_=ssf[:])
            o_ = of_[tl % 2]
            b.stt(o_[:], acc[:, tl, :], ssf[:, 0:1], gfbc[:], ALU.mult, ALU.mult)
            b.dma("sp", out_own[t0 + tl * 128: t0 + (tl + 1) * 128, :], o_[:], "oo%d" % (tl % 2))
        b.pop()
        b.pop()

    b.finish()
    return nc, b


def _rel_bucket_np(rel):
    nb, max_exact = 16, 8
    ret = (rel > 0).astype(np.int32) * nb
    n = np.abs(rel)
    lg = np.log(np.maximum(n, 1).astype(np.float32) / np.float32(max_exact)) / np.float32(math.log(1024 / 8))
    large = max_exact + (lg * np.float32(nb - max_exact)).astype(np.int32)
    large = np.minimum(large, nb - 1)
    return ret + np.where(n < max_exact, n, large)


def own_tokens(c, T):
    ns = T // 1024
    return np.concatenate([np.arange((8 * m + c) * 128, (8 * m + c + 1) * 128) for m in range(ns)])


def host_inputs(inp, T=T, stage="full"):
    f = lambda a: np.ascontiguousarray(np.asarray(a, dtype=np.float32))
    x = f(inp["x"])[0][:T]
    xT = f(x.T)
    w_in = f(inp["w_in"])[0]
    sm = f(inp["shift_mix"])[0]
    RWW = 1024
    o_zw, o_za, o_zg = 3 * RWW, 3 * RWW + 64, 3 * RWW + 128
    o_q = 3360
    o_ckv = o_q + 1024
    o_iq = o_ckv + 256
    o_ik = o_iq + 512
    o_iw = o_ik + 64
    o_g = o_iw + 8
    gm = f(inp["norm_mix_g"])[0]
    shared = {
        "w3q": f(w_in[:, o_q:o_q + 1024]), "w3iq": f(w_in[:, o_iq:o_iq + 512]), "w3iw": f(w_in[:, o_iw:o_iw + 8]),
        "relb": f(inp["rel_bias"]), "relb15": f(f(inp["rel_bias"])[15].reshape(16, 1)),
        "wukT": f(np.transpose(f(inp["w_uk"])[0], (2, 0, 1)).reshape(64, 16 * 256)),
    }
    wuv = f(inp["w_uv"])[0]
    wuvp = np.zeros((128, 16, 2, 128), np.float32)
    for h in range(16):
        for rc in range(2):
            wuvp[:, h, rc, (h % 2) * 64:(h % 2) * 64 + 64] = wuv[h, rc * 128:(rc + 1) * 128, :]
    shared["wuvp"] = f(wuvp.reshape(128, 16 * 2 * 128))
    if stage in ("mix", "full", "full_dbg"):
        shared.update({
            "w3g": f(w_in[:, o_g:o_g + 4096]), "wbr": f(inp["w_branch_rwkv"])[0], "wbd": f(inp["w_branch_dsa"])[0],
            "wout": f(inp["w_out"])[0],
        })
    if stage in ("full", "full_dbg"):
        sk = f(inp["peer_sub_keys"])[0]
        shared.update({
            "gffn": f(f(inp["norm_ffn_g"])[0].reshape(16, 128).T), "wpq": f(inp["w_peer_query"])[0],
            "skT": f(np.transpose(sk, (3, 0, 1, 2)).reshape(128, 16 * 128)),
            "puT": f(f(inp["peer_u"])[0].T), "pv": f(inp["peer_v"])[0],
            "gfin": f(f(inp["norm_final_g"]).reshape(1, D)),
        })
    per_core = []
    for c in range(NCORES):
        cols = []
        for base in (0, RWW, 2 * RWW):
            for h in range(2):
                hh = 2 * c + h
                cols.append(np.arange(base + hh * 64, base + hh * 64 + 64))
        cols.append(np.arange(o_zw, o_zw + 64)); cols.append(np.arange(o_za, o_za + 64))
        cols.append(np.arange(o_zg, o_zg + 160))
        cols.append(np.arange(o_ckv, o_ckv + 256)); cols.append(np.arange(o_ik, o_ik + 64))
        cols = np.concatenate(cols)
        assert cols.size == W1C
        w1 = f(w_in[:, cols])
        pu = np.zeros((64, 2, NPAR), np.float32)
        for h in range(2):
            hh = 2 * c + h
            ch = slice(hh * 64, hh * 64 + 64)
            pu[:, h, P_MIXR] = sm[0 * RWW:][ch]
            pu[:, h, P_MIXK] = sm[1 * RWW:][ch]
            pu[:, h, P_MIXV] = sm[2 * RWW:][ch]
            pu[:, h, P_W0] = f(inp["w0"])[0][ch]
            pu[:, h, P_A0] = f(inp["a0"])[0][ch]
            pu[:, h, P_KK] = f(inp["k_k"])[0][ch]
            pu[:, h, P_KA] = f(inp["k_a"])[0][ch]
            pu[:, h, P_RK] = f(inp["r_k"])[0].reshape(-1)[ch]
            pu[:, h, P_LNW] = f(inp["ln_x_w"])[0][ch]
            pu[:, h, P_LNB] = f(inp["ln_x_b"])[0][ch]
        mixl = np.zeros((64, 5), np.float32)
        mixl[:, 0] = sm[o_zw:o_zw + 64]; mixl[:, 1] = sm[o_za:o_za + 64]
        mixl[:, 2] = sm[o_zg:o_zg + 64]; mixl[:, 3] = sm[o_zg + 64:o_zg + 128]
        mixl[:32, 4] = sm[o_zg + 128:o_zg + 160]
        own = slice(c * 128, (c + 1) * 128)
        wlow = np.zeros((64, 5, 128), np.float32)
        wlow[:, 0] = f(inp["w_decay_up"])[0][:, own]
        wlow[:, 1] = f(inp["w_icl_up"])[0][:, own]
        wg = f(inp["w_gate_up"])[0][:, own]
        wlow[:, 2] = wg[0:64]; wlow[:, 3] = wg[64:128]; wlow[:32, 4] = wg[128:160]
        d = {
            "xT": xT, "w1": w1, "gmix": f(gm.reshape(16, 128).T),
            "pu": f(pu.reshape(64, 2 * NPAR)), "mixl": mixl, "wlow": f(wlow.reshape(64, 5 * 128)),
            "kvg": f(f(inp["kv_norm_g"])[0].reshape(2, 128).T),
        }
        if stage != "rwkv":
            toks = own_tokens(c, T)
            d["sel"] = f(np.tile((np.arange(8) == c).astype(np.float32)[None, :], (128, 1)))
            d["xTo"] = f(xT[:, toks])
            cap = np.full((128, 8, 128), -1e30, np.float32)
            qi = np.arange(128)[:, None]; si = np.arange(128)[None, :]
            for j in range(8):
                if j < c:
                    cap[:, j, :] = 3e38
                elif j == c:
                    cap[:, j, :] = np.where((si >= 64) & (qi < 64), -1e30, 3e38)
            d["cap"] = f(cap.reshape(128, 1024))
            xs_ = np.arange(NVEC)
            bk = _rel_bucket_np(xs_ - 127 - 128 * (8 + c))
            d["ohc"] = f((bk[None, :] == np.arange(32)[:, None]).astype(np.float32))
            d.update(shared)
        per_core.append(d)
    return per_core


def kernel(**inputs):
    nc, b = build("full")
    in_maps = host_inputs(inputs, T=T, stage="full")
    res = run_bass_kernel_spmd(nc, in_maps, core_ids=list(range(NCORES)))
    out = np.zeros((1, T, D), np.float32)
    for c in range(NCORES):
        out[0, own_tokens(c, T)] = res.results[c]["out_own"]
    return out
```

```python
import contextlib
import math
import numpy as np
import concourse.bass as bass
import concourse.mybir as mybir
from concourse.bass_utils import run_bass_kernel_spmd

F32 = mybir.dt.float32
BF16 = mybir.dt.bfloat16
ALU = mybir.AluOpType
AF = mybir.ActivationFunctionType
AX = mybir.AxisListType

WRITE_KEYS = ("out", "accum_out", "out_max", "out_indices")

NCORES = 8
D = 2048
T = 8192
TB = 256
NBLK = T // TB
EPS = 1e-6
GN_EPS = 64e-5
W1C = 992
NVEC = 2304
IDX_SCALE = 512 ** -0.5
SA_SCALE = 64 ** -0.5


class Eng:
    def __init__(self, name, h, sem):
        self.name, self.h, self.sem, self.count = name, h, sem, 0
        self.known = {}


class Builder:
    def __init__(self, nc):
        self.nc = nc
        self.es = contextlib.ExitStack()
        self.reg = {}
        self.engs = {}
        for name, h in (("pe", nc.tensor), ("act", nc.scalar), ("dve", nc.vector),
                        ("pool", nc.gpsimd), ("sp", nc.sync)):
            sem = self.es.enter_context(nc.semaphore("s_" + name))
            self.engs[name] = Eng(name, h, sem)
        self.dsem = {}
        self.n_ins = 0
        self.stack = [self.es]
        self.multi = {}

    def push(self):
        st = contextlib.ExitStack()
        self.stack.append(st)

    def pop(self):
        self.barrier()
        self.stack.pop().close()

    def sb(self, name, shape, dt=F32):
        used = self.__dict__.setdefault("_names", {})
        n = used.get(name, 0)
        used[name] = n + 1
        if n:
            name = "%s_v%d" % (name, n)
        return self.stack[-1].enter_context(self.nc.sbuf_tensor(name, list(shape), dt))

    def ps(self, name, shape, dt=F32):
        return self.es.enter_context(self.nc.psum_tensor(name, list(shape), dt))

    def dram(self, name, shape, dt=F32, kind="Internal"):
        return self.nc.dram_tensor(name, list(shape), dt, kind=kind)

    def _key(self, ap):
        name = ap.tensor.name
        nb = self.multi.get(name)
        if not nb:
            return [name]
        dims = list(ap.ap)
        row = dims[0][0]
        free = ap.offset % row
        span = 1 + sum((c - 1) * abs(st) for st, c in dims[1:])
        bs = row // nb
        return ["%s#%d" % (name, k) for k in range(free // bs, (free + span - 1) // bs + 1)]

    def _deps(self, reads, writes):
        deps = []
        for k in reads:
            r = self.reg.get(k)
            if r and r[0]:
                deps.append(r[0])
        for k in writes:
            r = self.reg.get(k)
            if r:
                if r[0]:
                    deps.append(r[0])
                deps.extend(r[1])
        return deps

    def _commit(self, tok, reads, writes):
        for k in reads:
            r = self.reg.setdefault(k, [None, []])
            r[1].append(tok)
            if len(r[1]) > 12:
                best = {}
                for s, v in r[1]:
                    if v > best.get(id(s), (None, 0))[1]:
                        best[id(s)] = (s, v)
                r[1] = list(best.values())
        for k in writes:
            self.reg[k] = [tok, []]

    def _wait(self, e, deps):
        best = {}
        for sem, val in deps:
            if sem is e.sem and e.name == "pe":
                continue
            kk = id(sem)
            if val > best.get(kk, (None, 0))[1]:
                best[kk] = (sem, val)
        for kk, (sem, val) in best.items():
            if e.known.get(kk, 0) < val:
                e.h.wait_ge(sem, val)
                e.known[kk] = val

    def _split(self, kw, extra_r, extra_w):
        reads, writes = list(extra_r), list(extra_w)
        for k, v in kw.items():
            if isinstance(v, bass.AP):
                (writes if k in WRITE_KEYS else reads).extend(self._key(v))
        return reads, writes

    def op(self, eng, fn, *args, R=(), W=(), **kw):
        e = self.engs[eng]
        reads, writes = self._split(kw, R, W)
        for a in args:
            if isinstance(a, bass.AP):
                reads.extend(self._key(a))
        self._wait(e, self._deps(reads, writes))
        ins = getattr(e.h, fn)(*args, **kw)
        e.count += 1
        ins.then_inc(e.sem, 1)
        self._commit((e.sem, e.count), reads, writes)
        self.n_ins += 1
        return ins

    def dma(self, q, out, in_, slot, R=(), W=(), **kw):
        e = self.engs[q]
        reads = self._key(in_) + list(R)
        writes = self._key(out) + list(W)
        self._wait(e, self._deps(reads, writes))
        if slot not in self.dsem:
            self.dsem[slot] = [self.es.enter_context(self.nc.semaphore("d_" + slot)), 0]
        s = self.dsem[slot]
        e.h.dma_start(out=out, in_=in_, **kw).then_inc(s[0], 16)
        s[1] += 16
        self._commit((s[0], s[1]), reads, writes)
        self.n_ins += 1

    def barrier(self):
        toks = [(e.sem, e.count) for e in self.engs.values() if e.count]
        toks += [(s[0], s[1]) for s in self.dsem.values() if s[1]]
        for e in self.engs.values():
            self._wait(e, list(toks))
        self.reg = {}

    def finish(self):
        self.barrier()
        self.es.close()

    def mm(self, out, lhsT, rhs, start=True, stop=True):
        return self.op("pe", "matmul", out=out, lhsT=lhsT, rhs=rhs, start=start, stop=stop)

    def tr(self, out, in_, ident):
        return self.op("pe", "transpose", out=out, in_=in_, identity=ident)

    def act(self, out, in_, func, eng="act", **kw):
        return self.op(eng, "activation", out=out, in_=in_, func=func, **kw)

    def tt(self, out, in0, in1, op, eng="dve"):
        return self.op(eng, "tensor_tensor", out=out, in0=in0, in1=in1, op=op)

    def ts(self, out, in0, s1, op0, s2=None, op1=None, eng="dve", **kw):
        if op1 is None:
            return self.op(eng, "tensor_scalar", out=out, in0=in0, scalar1=s1, scalar2=None, op0=op0, **kw)
        return self.op(eng, "tensor_scalar", out=out, in0=in0, scalar1=s1, scalar2=s2, op0=op0, op1=op1, **kw)

    def stt(self, out, in0, scalar, in1, op0, op1, eng="dve", **kw):
        return self.op(eng, "scalar_tensor_tensor", out=out, in0=in0, scalar=scalar, in1=in1,
                       op0=op0, op1=op1, **kw)

    def copy(self, out, in_, eng="dve"):
        if eng == "act":
            return self.op("act", "activation", out=out, in_=in_, func=AF.Copy)
        return self.op(eng, "tensor_copy", out=out, in_=in_)

    def memset(self, ap, val, eng="pool"):
        e = self.engs[eng]
        k = self._key(ap)
        self._wait(e, self._deps([], k))
        ins = e.h.memset(ap, val)
        e.count += 1
        ins.then_inc(e.sem, 1)
        self._commit((e.sem, e.count), [], k)
        self.n_ins += 1
        return ins


P_MIXR, P_MIXK, P_MIXV, P_W0, P_A0, P_KK, P_KA, P_RK, P_LNW, P_LNB = range(10)
NPAR = 10


def build(stage="rwkv", nblk=None, T=T, taps=()):
    nblk = (T // TB) if nblk is None else nblk
    nc = bass.Bass("TRN2", target_bir_lowering=False)
    b = Builder(nc)
    IN = lambda name, shape: nc.dram_tensor(name, list(shape), F32, kind="ExternalInput").ap()
    xT = IN("xT", [D, T])
    w1 = IN("w1", [D, W1C])
    gmix = IN("gmix", [128, 16])
    pu = IN("pu", [64, 2 * NPAR])
    mixl = IN("mixl", [64, 5])
    wlow = IN("wlow", [64, 5 * 128])
    kvg = IN("kvg", [128, 2])
    rw_loc = (nc.dram_tensor("rw_loc", [128, T], F32, kind="ExternalOutput").ap() if stage == "rwkv"
              else nc.dram_tensor("rw_loc", [128, T], F32).ap())
    cT_d = nc.dram_tensor("cT_d", [256, T], BF16, kind="Internal").ap()
    ikT_d = nc.dram_tensor("ikT_d", [64, T], BF16, kind="Internal").ap()

    ident = b.sb("ident", [128, 128])
    b.op("pool", "iota", ident[:], [[1, 128]], base=0, channel_multiplier=-1,
         allow_small_or_imprecise_dtypes=True, W=["ident"])
    b.ts(ident[:], ident[:], 0.0, ALU.is_equal)
    ones_b = b.sb("ones_b", [128, 128], BF16)
    b.memset(ones_b[:], 1.0)
    epst = b.sb("epst", [128, 1]); b.memset(epst[:], EPS)
    gmixt = b.sb("gmixt", [128, 16]); b.dma("sp", gmixt[:], gmix, "c0_1")
    b.push()
    dmat = b.sb("Wm", [64, 8, 64])
    b.op("pool", "iota", dmat[:], [[0, 8], [-1, 64]], base=0, channel_multiplier=1,
         allow_small_or_imprecise_dtypes=True, W=["Wm"])
    MS = b.sb("MS", [64, 8, 64]); MST = b.sb("MST", [64, 8, 64]); MIT = b.sb("MIT", [64, 8, 64])
    I8 = b.sb("I8", [64, 8, 64])
    b.ts(MS[:], dmat[:], 0.0, ALU.is_gt)
    b.ts(MST[:], dmat[:], 0.0, ALU.is_lt)
    b.ts(MIT[:], dmat[:], 0.0, ALU.is_le)
    b.ts(I8[:], dmat[:], 0.0, ALU.is_equal)
    ones64 = b.sb("ones64", [64, 64])
    b.memset(ones64[:], 1.0)
    mean64 = b.sb("mean64", [64, 64])
    b.memset(mean64[:], 1.0 / 64)
    segm = b.sb("segm", [64, 2 * TB])
    b.memset(segm[:], 1.0)
    for u in range(2 * TB // 64):
        b.memset(segm[:, u * 64:u * 64 + 1], 0.0)
    gnepst = b.sb("gnepst", [128, 1]); b.memset(gnepst[:], GN_EPS)

    put = b.sb("put", [64, 2, NPAR]); b.dma("sp", put[:], pu.rearrange("p (h n) -> p h n", h=2), "c0_2")
    mixlt = b.sb("mixlt", [64, 5]); b.dma("sp", mixlt[:], mixl, "c0_3")
    wlowt = b.sb("wlowt", [64, 5, 128]); b.dma("sp", wlowt[:], wlow.rearrange("p (j n) -> p j n", j=5), "c0_4")
    kvgt = b.sb("kvgt", [128, 2]); b.dma("sp", kvgt[:], kvg, "c0_5")
    mix11 = b.sb("mix11", [64, 11])
    for h in range(2):
        b.copy(mix11[:, 0 + h:1 + h], put[:, h, P_MIXR:P_MIXR + 1])
        b.copy(mix11[:, 2 + h:3 + h], put[:, h, P_MIXK:P_MIXK + 1])
        b.copy(mix11[:, 4 + h:5 + h], put[:, h, P_MIXV:P_MIXV + 1])
    b.copy(mix11[:, 6:11], mixlt[:])

    PSA = b.ps("PSA", [128, 2048]); PSB = b.ps("PSB", [128, 2048])
    b.multi["PSA"] = 4; b.multi["PSB"] = 4
    banks = [PSA[:, i * 512:(i + 1) * 512] for i in range(4)] + [PSB[:, i * 512:(i + 1) * 512] for i in range(4)]
    wide = [PSA[:, 0:1024], PSA[:, 1024:2048], PSB[:, 0:1024]]
    bank_i = [0]

    def bank():
        bank_i[0] = (bank_i[0] + 1) % 8
        return banks[bank_i[0]]

    w1b = b.sb("w1b", [128, 16, W1C], BF16)

    xs = [b.sb("xs%d" % i, [128, 16, TB]) for i in range(2)]
    for c in range(16):
        st = xs[c % 2][:].rearrange("p c t -> p (c t)")[:, 0:W1C]
        b.dma("sp", st, w1[c * 128:(c + 1) * 128, :], "x%d" % (c % 2))
        b.ts(w1b[:, c, :], st, gmixt[:, c:c + 1], ALU.mult, eng=("pool" if c % 2 else "dve"))
    xb = b.sb("xb", [128, 16, TB], BF16)
    sq = b.sb("sq", [128, 16, TB], BF16)
    rstd = b.sb("rstd", [128, TB])
    PRt = b.sb("PRt", [64, 11, TB])
    b.memset(PRt[:], 0.0)
    pcol = b.sb("pcol", [64, 11, 1]); b.memset(pcol[:], 0.0)
    Z = b.sb("Z", [64, 11, TB])
    ckv = b.sb("ckv", [128, 2, TB]); ckv2 = b.sb("ckv2", [128, 2, TB], BF16)
    cTb = b.sb("cTb", [128, 2, TB], BF16)
    ikb = b.sb("ikb", [64, TB], BF16)

    def t2(name, dt=F32):
        return b.sb(name, [64, 2, TB], dt)

    tzs = b.sb("tzs", [64, 5, TB])
    b.memset(tzs[:], 0.0)
    Aa = t2("Aa"); Gt = t2("Gt"); KK = t2("KK"); KKN = t2("KKN"); K4 = t2("K4")
    TMP = t2("TMP"); TMP2 = t2("TMP2"); BON = t2("BON"); CL = t2("CL"); LW = t2("LW")
    Pm = t2("Pm"); RpT = t2("RpT"); ApT = t2("ApT"); BmT = t2("BmT"); KmT = t2("KmT")
    BpT = t2("BpT"); KpT = t2("KpT"); KA = t2("KA")
    PL = b.sb("PL", [64, 8])
    ApM = b.sb("ApM", [64, 8, 64]); BpM = b.sb("BpM", [64, 8, 64]); KpM = b.sb("KpM", [64, 8, 64])
    VM = b.sb("VM", [64, 8, 64])
    Nb = [b.sb("Nb%d" % i, [64, 8, 64]) for i in range(2)]; Nb0 = Nb[0]
    NTb = [b.sb("NTb%d" % i, [64, 8, 64]) for i in range(2)]; NTb0 = NTb[0]
    TTm = b.sb("TTm", [64, 8, 64]); BTs = b.sb("BTs", [64, 8, 64]); CTs = b.sb("CTs", [64, 8, 64])
    ETs = b.sb("ETs", [64, 8, 64]); Wm = dmat; BVm = b.sb("BVm", [64, 8, 64])
    UVm = b.sb("UVm", [64, 8, 64]); QtT = b.sb("QtT", [64, 8, 64]); GTm = b.sb("GTm", [64, 8, 64])
    ST = b.sb("ST", [64, 2, 64]); b.memset(ST[:], 0.0)
    Yg = t2("Yg"); Yc = t2("Yc"); RWo = Yc

    GRP = [(0, 64), (64, 64), (128, 64), (192, 64), (256, 64), (320, 64),
           (384, 64), (448, 64), (512, 64), (576, 64), (640, 32)]
    xTv = xT.rearrange("(c p) t -> p c t", p=128)

    def U(h, cc):
        return slice(cc * 64, (cc + 1) * 64)

    for blk in range(nblk):
        t0 = blk * TB
        xs_ = xs[blk % 2]
        b.dma("sp", xs_[:], xTv[:, :, t0:t0 + TB], "x%d" % (blk % 2))
        b.act(sq[:], xs_[:], AF.Square)
        b.copy(xb[:], xs_[:], eng="pool")
        pss = bank()
        for c in range(16):
            b.mm(pss[:, 0:TB], ones_b[:], sq[:, c, :], start=(c == 0), stop=(c == 15))
        b.act(rstd[:], pss[:, 0:TB], AF.Sqrt, scale=1.0 / D, bias=epst[:])
        b.op("dve", "reciprocal", out=rstd[:], in_=rstd[:])
        for j, (c0, wd) in enumerate(GRP):
            pb = bank()
            for c in range(16):
                b.mm(pb[0:wd, 0:TB], w1b[:, c, c0:c0 + wd], xb[:, c, :], start=(c == 0), stop=(c == 15))
            b.tt(PRt[0:wd, j, :], pb[0:wd, 0:TB], rstd[0:wd, :], ALU.mult,
                 eng="dve")
        for j in range(2):
            pb = bank()
            for c in range(16):
                b.mm(pb[:, 0:TB], w1b[:, c, 672 + j * 128:672 + (j + 1) * 128], xb[:, c, :],
                     start=(c == 0), stop=(c == 15))
            b.tt(ckv[:, j, :], pb[:, 0:TB], rstd[:], ALU.mult)
        pb = bank()
        for c in range(16):
            b.mm(pb[0:64, 0:TB], w1b[:, c, 928:992], xb[:, c, :], start=(c == 0), stop=(c == 15))
        b.tt(ikb[:], pb[0:64, 0:TB], rstd[0:64, :], ALU.mult)
        b.dma("pool", ikT_d[:, t0:t0 + TB], ikb[:], "oik")
        b.act(ckv2[:], ckv[:], AF.Square)
        pc = bank()
        for j in range(2):
            b.mm(pc[:, 0:TB], ones_b[:], ckv2[:, j, :], start=(j == 0), stop=(j == 1))
        if blk == 0:
            build.crs = b.sb("crs", [128, TB])
        crs = build.crs
        b.act(crs[:], pc[:, 0:TB], AF.Sqrt, scale=1.0 / 256, bias=epst[:])
        b.op("dve", "reciprocal", out=crs[:], in_=crs[:])
        for j in range(2):
            b.stt(cTb[:, j, :], ckv[:, j, :], kvgt[:, j:j + 1], crs[:], ALU.mult, ALU.mult)
        b.dma("pool", cT_d.rearrange("(j p) t -> p j t", p=128)[:, :, t0:t0 + TB], cTb[:], "oc")

        b.tt(Z[:, :, 1:TB], PRt[:, :, 0:TB - 1], PRt[:, :, 1:TB], ALU.subtract)
        b.tt(Z[:, :, 0:1], pcol[:], PRt[:, :, 0:1], ALU.subtract)
        b.tt(Z[:], Z[:], mix11[:].unsqueeze(2).to_broadcast([64, 11, TB]), ALU.mult)
        b.tt(Z[:], Z[:], PRt[:], ALU.add)
        b.copy(pcol[:], PRt[:, :, TB - 1:TB], eng="pool")
        ZR = Z[:, 0:2, :]; ZK = Z[:, 2:4, :]; ZV = Z[:, 4:6, :]
        b.act(tzs[:, 0, :], Z[:, 6, :], AF.Tanh)
        b.copy(tzs[:, 1, :], Z[:, 7, :], eng="pool")
        b.act(tzs[:, 2:4, :], Z[:, 8:10, :], AF.Sigmoid)
        b.act(tzs[0:32, 4, :], Z[0:32, 10, :], AF.Sigmoid)
        for h in range(2):
            hs = slice(h * 64, (h + 1) * 64)
            pb = bank()
            b.mm(pb[0:64, 0:TB], wlowt[:, 0, hs], tzs[:, 0, :])
            b.act(LW[:, h, :], pb[0:64, 0:TB], AF.Sigmoid, bias=put[:, h, P_W0:P_W0 + 1])
            b.mm(pb[0:64, TB:2 * TB], wlowt[:, 1, hs], tzs[:, 1, :])
            b.act(Aa[:, h, :], pb[0:64, TB:2 * TB], AF.Sigmoid, bias=put[:, h, P_A0:P_A0 + 1])
            pb2 = bank()
            b.mm(pb2[0:64, 0:TB], wlowt[:, 2, hs], tzs[:, 2, :], start=True, stop=False)
            b.mm(pb2[0:64, 0:TB], wlowt[:, 3, hs], tzs[:, 3, :], start=False, stop=False)
            b.mm(pb2[0:64, 0:TB], wlowt[0:32, 4, hs], tzs[0:32, 4, :], start=False, stop=True)
            b.copy(Gt[:, h, :], pb2[0:64, 0:TB], eng="act")
            b.ts(KK[:, h, :], ZK[:, h, :], put[:, h, P_KK:P_KK + 1], ALU.mult)
            b.ts(TMP[:, h, :], Aa[:, h, :], -1.0, ALU.add, put[:, h, P_KA:P_KA + 1], ALU.mult)
            b.stt(K4[:, h, :], TMP[:, h, :], 1.0, ZK[:, h, :], ALU.add, ALU.mult)
            b.stt(TMP2[:, h, :], ZR[:, h, :], put[:, h, P_RK:P_RK + 1], K4[:, h, :], ALU.mult, ALU.mult)
        b.tt(TMP[:], KK[:], KK[:], ALU.mult)
        pn = bank()
        b.mm(pn[0:64, :], ones64[:], TMP[:].rearrange("p h t -> p (h t)"))
        b.act(KKN[:].rearrange("p h t -> p (h t)"), pn[0:64, :], AF.Sqrt)
        b.ts(KKN[:], KKN[:], 1e-12, ALU.max)
        b.op("dve", "reciprocal", out=KKN[:], in_=KKN[:])
        b.tt(KKN[:], KKN[:], KK[:], ALU.mult)
        pbn = bank()
        b.mm(pbn[0:64, :], ones64[:], TMP2[:].rearrange("p h t -> p (h t)"))
        b.tt(BON[:].rearrange("p h t -> p (h t)"), pbn[0:64, :], Z[:, 4:6, :].rearrange("p h t -> p (h t)"), ALU.mult)
        b.ts(LW[:], LW[:], -0.6065306597126334, ALU.mult)
        b.op("dve", "tensor_tensor_scan", out=CL[:].rearrange("p h t -> p (h t)"), data0=segm[:],
             data1=LW[:].rearrange("p h t -> p (h t)"), initial=0.0, op0=ALU.mult, op1=ALU.add)
        CLv = CL[:].rearrange("p h (c t) -> p (h c) t", t=64)
        b.act(Pm[:], CL[:], AF.Exp)
        b.tt(RpT[:], ZR, Pm[:], ALU.mult)
        b.copy(PL[:], Pm[:].rearrange("p h (c t) -> p (h c) t", t=64)[:, :, 63], eng="pool")
        b.tt(TMP[:], CL[:], LW[:], ALU.subtract)
        b.act(Pm[:], TMP[:], AF.Exp)
        b.stt(ApT[:], KKN[:], -1.0, Pm[:], ALU.mult, ALU.mult)
        b.act(Pm[:], CL[:], AF.Exp, scale=-1.0)
        b.tt(KA[:], KKN[:], Aa[:], ALU.mult)
        b.tt(BmT[:], KA[:], Pm[:], ALU.mult)
        b.tt(KmT[:], K4[:], Pm[:], ALU.mult)
        b.tt(TMP[:].rearrange("p h (c t) -> p (h c) t", t=64),
             CLv[:, :, 63:64].to_broadcast([64, 8, 64]), CLv, ALU.subtract)
        b.act(Pm[:], TMP[:], AF.Exp)
        b.tt(BpT[:], KA[:], Pm[:], ALU.mult)
        b.tt(KpT[:], K4[:], Pm[:], ALU.mult)

        units = [(h, cc) for h in range(2) for cc in range(4)]

        def FM(X, h, cc):
            return X[:, h, cc * 64:(cc + 1) * 64]

        for X, dst in ((ApT, ApM), (BpT, BpM), (KpT, KpM), (None, VM)):
            pb = bank()
            for u, (h, cc) in enumerate(units):
                src = FM(X, h, cc) if X is not None else Z[:, 4 + h, cc * 64:(cc + 1) * 64]
                b.tr(pb[0:64, u * 64:(u + 1) * 64], src, ident[0:64, 0:64])
            b.copy(dst[:].rearrange("p u k -> p (u k)"), pb[0:64, :], eng="act")

        def prod(L, Rr, mask, dst, eng="dve"):
            pb = bank()
            for u, (h, cc) in enumerate(units):
                b.mm(pb[0:64, u * 64:(u + 1) * 64], FM(L, h, cc), FM(Rr, h, cc))
            b.tt(dst[:].rearrange("p u k -> p (u k)"), pb[0:64, :], mask[:].rearrange("p u k -> p (u k)"),
                 ALU.mult, eng=eng)

        N, NT = Nb[0], NTb[0]
        prod(ApT, BmT, MS, N)
        prod(BmT, ApT, MST, NT)
        prod(KmT, ApT, MST, BTs)
        prod(BmT, RpT, MIT, CTs)
        prod(KmT, RpT, MIT, ETs)
        b.tt(TTm[:], NT[:], I8[:], ALU.add)
        for i in range(1, 6):
            N2, NT2 = Nb[i % 2], NTb[i % 2]
            pb = bank()
            for u in range(8):
                b.mm(pb[0:64, u * 64:(u + 1) * 64], NT[:, u, :], N[:, u, :])
            b.copy(N2[:].rearrange("p u k -> p (u k)"), pb[0:64, :], eng="act")
            if i < 5:
                pb2 = bank()
                for u in range(8):
                    b.mm(pb2[0:64, u * 64:(u + 1) * 64], N[:, u, :], NT[:, u, :])
                b.copy(NT2[:].rearrange("p u k -> p (u k)"), pb2[0:64, :])
            pb3 = bank()
            for u in range(8):
                b.mm(pb3[0:64, u * 64:(u + 1) * 64], N2[:, u, :], TTm[:, u, :])
            b.tt(TTm[:].rearrange("p u k -> p (u k)"), TTm[:].rearrange("p u k -> p (u k)"), pb3[0:64, :], ALU.add)
            N, NT = N2, NT2

        def prod2(Lm, Rm, dst, eng="act"):
            pb = bank()
            for u in range(8):
                b.mm(pb[0:64, u * 64:(u + 1) * 64], Lm[:, u, :], Rm[:, u, :])
            if dst is not None:
                b.copy(dst[:].rearrange("p u k -> p (u k)"), pb[0:64, :], eng=eng)
            return pb

        prod2(TTm, ApM, Wm)
        prod2(BTs, VM, BVm, eng="dve")
        prod2(TTm, BVm, UVm)
        pq = prod2(Wm, CTs, None)
        b.tt(QtT[:].rearrange("p (h c) t -> p h (c t)", h=2), pq[0:64, :].rearrange("p (h x) -> p h x", h=2),
             RpT[:], ALU.add)
        prod2(Wm, BpM, GTm, eng="dve")
        pY = bank(); pS = bank()
        for cc in range(4):
            for h in range(2):
                u = h * 4 + cc
                us = slice(u * 64, (u + 1) * 64)
                b.mm(pY[0:64, us], UVm[:, u, :], CTs[:, u, :], start=True, stop=False)
                b.mm(pY[0:64, us], VM[:, u, :], ETs[:, u, :], start=False, stop=False)
                b.mm(pY[0:64, us], ST[:, h, :], QtT[:, u, :], start=False, stop=True)
                b.mm(pS[0:64, us], BpM[:, u, :], UVm[:, u, :], start=True, stop=False)
                b.mm(pS[0:64, us], KpM[:, u, :], VM[:, u, :], start=False, stop=False)
                b.mm(pS[0:64, us], GTm[:, u, :], ST[:, h, :], start=False, stop=True)
            for h in range(2):
                u = h * 4 + cc
                us = slice(u * 64, (u + 1) * 64)
                b.stt(ST[:, h, :], ST[:, h, :], PL[:, u:u + 1], pS[0:64, us], ALU.mult, ALU.add)
        Ygf = Yg[:].rearrange("p h t -> p (h t)")
        b.copy(Ygf, pY[0:64, :], eng="act")
        pm = bank()
        b.mm(pm[0:64, :], mean64[:], Ygf)
        Ycf = Yc[:].rearrange("p h t -> p (h t)")
        b.tt(Ycf, Ygf, pm[0:64, :], ALU.subtract)
        b.tt(TMP[:], Yc[:], Yc[:], ALU.mult)
        pv = bank()
        b.mm(pv[0:64, :], mean64[:], TMP[:].rearrange("p h t -> p (h t)"))
        b.act(TMP2[:].rearrange("p h t -> p (h t)"), pv[0:64, :], AF.Sqrt, bias=gnepst[0:64, :])
        b.op("dve", "reciprocal", out=TMP2[:], in_=TMP2[:])
        b.tt(Yc[:], Yc[:], TMP2[:], ALU.mult)
        for h in range(2):
            b.ts(Yc[:, h, :], Yc[:, h, :], put[:, h, P_LNW:P_LNW + 1], ALU.mult,
                 put[:, h, P_LNB:P_LNB + 1], ALU.add)
        b.tt(Yc[:], Yc[:], BON[:], ALU.add)
        b.tt(RWo[:], Yc[:], Gt[:], ALU.mult)
        b.dma("pool", rw_loc.rearrange("(h p) t -> p h t", p=64)[:, :, t0:t0 + TB], RWo[:], "orw")
        if blk == 0 and taps:
            loc = dict(locals())
            for nm in taps:
                tl = loc[nm]
                shp = list(tl.shape)
                o = nc.dram_tensor("tap_" + nm, shp, F32, kind="ExternalOutput").ap()
                b.dma("pool", o, tl[:], "otap")

    b.pop()
    if stage == "rwkv":
        b.finish()
        return nc, b

    NS = T // 1024
    NTOK = NS * 128
    NKT = T // 128
    NCH = min(512, NTOK)
    DT = lambda name, shape, dt=F32: nc.dram_tensor(name, list(shape), dt).ap()

    sel = IN("sel", [128, 8])
    rw_all = DT("rw_all", [1024, T])
    rwo_d = DT("rwo_d", [1024, NTOK], BF16)
    e = b.engs["pool"]
    b._wait(e, b._deps(["rw_loc"], ["rw_all"]))
    csem = b.es.enter_context(nc.semaphore("csem"))
    nc.gpsimd.collective_compute("AllGather", ALU.bypass, replica_groups=[list(range(NCORES))],
                                 ins=[rw_loc.opt()], outs=[rw_all.opt()]).then_inc(csem)
    b._commit((csem, 1), ["rw_loc"], ["rw_all"])
    b.push()
    selt = b.sb("selt", [128, 8]); b.dma("sp", selt[:], sel, "c0_6")
    gblk = [b.sb("gblk%d" % i, [128, 8, 1024]) for i in range(2)]
    rwo = b.sb("rwo", [128, 8, NS, 128])
    rwob = b.sb("rwob", [128, 8, NS, 128], BF16)
    rav = rw_all.rearrange("(c p) t -> p c t", p=128)
    for m in range(NS):
        g = gblk[m % 2]
        b.dma("pool", g[:], rav[:, :, m * 1024:(m + 1) * 1024], "g%d" % (m % 2))
        gv = g[:].rearrange("p c (j i) -> p c j i", j=8)
        b.ts(rwo[:, :, m, :], gv[:, :, 0, :], selt[:, 0:1], ALU.mult)
        for j in range(1, 8):
            b.stt(rwo[:, :, m, :], gv[:, :, j, :], selt[:, j:j + 1], rwo[:, :, m, :], ALU.mult, ALU.add)
    b.copy(rwob[:], rwo[:], eng="act")
    b.dma("sp", rwo_d.rearrange("(c p) t -> p c t", p=128), rwob[:].rearrange("p c m i -> p c (m i)"), "og")
    b.pop()

    xTo = IN("xTo", [D, NTOK])
    w3q = IN("w3q", [D, 1024]); w3iq = IN("w3iq", [D, 512]); w3iw = IN("w3iw", [D, 8])
    q_d = DT("q_d", [64, 16 * NTOK], BF16)
    xbo_d = DT("xbo_d", [D, NTOK], BF16)
    rso_d = DT("rso_d", [128, NTOK])
    iq_d = DT("iq_d", [64, 8 * NTOK], BF16)
    identb = b.sb("identb", [128, 128], BF16)
    b.copy(identb[:], ident[:])
    iwTM = b.sb("iwTM", [128, NS, 8])
    xov = xTo.rearrange("(c p) t -> p c t", p=128)
    b.push()
    xbo = b.sb("xbo", [128, 16, NTOK], BF16)
    sqo = b.sb("sqo", [128, 16, NTOK], BF16)
    rso = b.sb("rso", [128, NTOK])
    b.push()
    xo = b.sb("xo", [128, 16, NTOK])
    b.dma("sp", xo[:], xov, "x0")
    b.act(sqo[:], xo[:], AF.Square)
    for c in range(16):
        b.ts(xbo[:, c, :], xo[:, c, :], gmixt[:, c:c + 1], ALU.mult, eng=("pool" if c % 2 else "dve"))
    b.pop()
    qTs = b.sb("qTs", [64, 16, NTOK], BF16)
    iqTs = b.sb("iqTs", [64, 8, NTOK], BF16)
    iwTs = b.sb("iwTs", [8, NTOK])
    wst3 = [b.sb("wst3_%d" % i, [128, 16, 64]) for i in range(2)]
    wb3 = [b.sb("wb3_%d" % i, [128, 16, 64], BF16) for i in range(2)]
    for n0 in range(0, NTOK, NCH):
        pss = bank()
        for c in range(16):
            b.mm(pss[:, 0:NCH], ones_b[:], sqo[:, c, n0:n0 + NCH], start=(c == 0), stop=(c == 15))
        b.act(rso[:, n0:n0 + NCH], pss[:, 0:NCH], AF.Sqrt, scale=1.0 / D, bias=epst[:])
    b.op("dve", "reciprocal", out=rso[:], in_=rso[:])
    groups = [(w3q, h * 64, 64, qTs, h) for h in range(16)] + [(w3iq, h * 64, 64, iqTs, h) for h in range(8)] \
        + [(w3iw, 0, 8, iwTs, None)]
    for gi, (wsrc, c0, wd, dst, hh) in enumerate(groups):
        st = wst3[gi % 2]; wb = wb3[gi % 2]
        b.dma("sp", st[:, :, 0:wd], wsrc.rearrange("(c p) n -> p c n", p=128)[:, :, c0:c0 + wd], "w3_%d" % (gi % 2))
        b.copy(wb[:, :, 0:wd], st[:, :, 0:wd], eng="act")
        for n0 in range(0, NTOK, NCH):
            pb = bank()
            for c in range(16):
                b.mm(pb[0:wd, 0:NCH], wb[:, c, 0:wd], xbo[:, c, n0:n0 + NCH], start=(c == 0), stop=(c == 15))
            o = dst[:, hh, n0:n0 + NCH] if hh is not None else dst[:, n0:n0 + NCH]
            b.tt(o, pb[0:wd, 0:NCH], rso[0:wd, n0:n0 + NCH], ALU.mult)
    b.dma("sp", xbo_d.rearrange("(c p) t -> p c t", p=128), xbo[:], "oq_1")
    b.dma("sp", rso_d, rso[:], "oq_2")
    b.dma("sp", q_d.rearrange("p (h t) -> p h t", h=16), qTs[:], "oq_3")
    b.dma("sp", iq_d.rearrange("p (h t) -> p h t", h=8), iqTs[:], "oq_4")
    for m in range(NS):
        pb = bank()
        b.tr(pb[:, 0:8], iwTs[:, m * 128:(m + 1) * 128], ident[0:8, 0:8])
        b.ts(iwTM[:, m, :], pb[:, 0:8], IDX_SCALE, ALU.mult)
    b.pop()

    capi = IN("cap", [128, 1024])
    ohc = IN("ohc", [32, NVEC])
    relb = IN("relb", [32, 16]); relb15 = IN("relb15", [16, 1])
    wukT = IN("wukT", [64, 16 * 256]); wuvp = IN("wuvp", [128, 16 * 2 * 128])
    vec_d = DT("vec_d", [16, NVEC])
    EBn_d = DT("EBn_d", [16 * 128, 2048], BF16)
    dsa_d = DT("dsa_d", [1024, NTOK], BF16)
    b.push()
    cT = b.sb("cT", [128, 2, T], BF16)
    ikT = b.sb("ikT", [64, T], BF16)
    cTM = b.sb("cTM", [128, NKT, 256], BF16)
    sc = b.sb("sc", [128, T])
    if T >= 2048:
        b.multi["sc"] = T // 1024
    msel = b.sb("msel", [128, T], BF16)
    MT = sc[:].bitcast(BF16)[:, 0:NKT * 128].rearrange("p (k q) -> p k q", q=128)
    capt = b.sb("capt", [128, 1024])
    wukb = b.sb("wukb", [64, 16, 256], BF16)
    wuvb = b.sb("wuvb", [128, 16, 2, 128], BF16)
    dsaTs = [b.sb("dsaTs%d" % i, [128, 8, 128], BF16) for i in range(2)]
    Jm = b.sb("Jm", [128, 128])
    b.op("pool", "iota", Jm[:], [[1, 128]], base=-127, channel_multiplier=1,
         allow_small_or_imprecise_dtypes=True, W=["Jm"])
    b.ts(Jm[:], Jm[:], 0.0, ALU.is_equal)
    b.dma("sp", cT[:], cT_d.rearrange("(j p) t -> p j t", p=128), "dxa")
    b.dma("sp", ikT[:], ikT_d, "dxb")
    b.dma("sp", capt[:], capi, "c0_7")
    b.push()
    scw = sc[:, 0:4096] if T >= 4096 else b.sb("scw", [128, 4096])[:, :]
    b.dma("sp", scw[0:64, :], wukT, "w3_0")
    b.copy(wukb[:].rearrange("p h r -> p (h r)"), scw[0:64, :], eng="act")
    b.dma("sp", scw, wuvp, "w3_1")
    b.copy(wuvb[:].rearrange("p h c n -> p (h c n)"), scw, eng="act")
    b.pop()
    pbf = lambda pb: pb[:].bitcast(BF16)
    for k0 in range(0, NKT, 4):
        pb = bank()
        v = pbf(pb)
        for k in range(4):
            for rc in range(2):
                b.tr(v[:, (k * 2 + rc) * 128:(k * 2 + rc + 1) * 128], cT[:, rc, (k0 + k) * 128:(k0 + k + 1) * 128], identb[:])
        b.copy(cTM[:, k0:k0 + 4, :].rearrange("p k r -> p (k r)"), v[:, 0:1024], eng="act")
    b.push()
    relbt = b.sb("relbt", [32, 16]); b.dma("sp", relbt[:], relb, "c0_8")
    nb15 = b.sb("nb15", [16, 1]); b.dma("sp", nb15[:], relb15, "c0_9")
    b.ts(nb15[:], nb15[:], -1.0, ALU.mult)
    oht = b.sb("oht", [32, NVEC]); b.dma("sp", oht[:], ohc, "c0_10")
    vecs = b.sb("vecs", [16, NVEC])
    for x0 in range(0, NVEC, 512):
        xw = min(512, NVEC - x0)
        pb = bank()
        b.mm(pb[0:16, 0:xw], relbt[:], oht[:, x0:x0 + xw])
        b.act(vecs[:, x0:x0 + xw], pb[0:16, 0:xw], AF.Exp, bias=nb15[:])
    b.dma("sp", vec_d, vecs[:], "ov")
    Tp = [b.sb("Tp%d" % i, [128, 16, 128]) for i in range(2)]
    EBs = [b.sb("EBs%d" % i, [128, 16, 128], BF16) for i in range(2)]
    for jj in range(16):
        tp = Tp[jj % 2]; ebs = EBs[jj % 2]
        hank = bass.AP(vec_d.tensor, 128 * jj, [[1, 128], [NVEC, 16], [1, 128]])
        b.dma("sp", tp[:], hank, "hk%d" % (jj % 2), R=["vec_d"])
        for h4 in range(4):
            pb = bank()
            for k in range(4):
                b.mm(pb[:, k * 128:(k + 1) * 128], tp[:, h4 * 4 + k, :], Jm[:])
            b.copy(ebs[:, h4 * 4:h4 * 4 + 4, :].rearrange("p h q -> p (h q)"), pb[:], eng="act")
        b.dma("sp", EBn_d[jj * 128:(jj + 1) * 128, :], ebs[:].rearrange("p h q -> p (h q)"), "oeb%d" % (jj % 2))
    b.pop()

    qs = b.sb("qs", [64, 16, 128], BF16); iqs = b.sb("iqs", [64, 8, 128], BF16)
    qlT = b.sb("qlT", [128, 2, 2048], BF16); olT = b.sb("olT", [128, 2, 2048], BF16)
    Rt = [b.sb("Rt%d" % i, [128, 1024]) for i in range(2)]
    wabs = b.sb("wabs", [128, 8]); wsgn = b.sb("wsgn", [128, 8])
    Et = [b.sb("Et%d" % i, [128, 4, 128], BF16) for i in range(3)]
    EBt = [b.sb("EBt%d" % i, [128, 4, 128], BF16) for i in range(2)]
    rden = b.sb("rden", [128, 512])
    lo = b.sb("lo", [128, 1]); mid = b.sb("mid", [128, 1]); cnt = b.sb("cnt", [128, 1]); gg = b.sb("gg", [128, 1])
    q_dv = q_d.rearrange("p (h t) -> p h t", h=16); iq_dv = iq_d.rearrange("p (h t) -> p h t", h=8)
    EBn_v = EBn_d.rearrange("(j s) (h q) -> j s h q", s=128, h=16)
    bank5 = [0]

    def bank_lo():
        bank5[0] = (bank5[0] + 1) % 5
        return banks[bank5[0]]

    po0, po1, pdn = banks[5], banks[6], banks[7]
    for m in range(NS):
        nkt = 8 * m + 8
        S = nkt * 128
        tsl = slice(m * 128, (m + 1) * 128)
        b.dma("sp", qs[:], q_dv[:, :, tsl], "qsA")
        b.dma("sp", iqs[:], iq_dv[:, :, tsl], "qsB")
        b.ts(wsgn[:], iwTM[:, m, :], 0.0, ALU.is_ge, 2.0, ALU.mult)
        b.ts(wsgn[:], wsgn[:], -1.0, ALU.add)
        b.tt(wabs[:], iwTM[:, m, :], wsgn[:], ALU.mult)
        wi = 0
        for g in range(S // 1024):
            gsl = slice(g * 1024, (g + 1) * 1024)
            for h in range(8):
                pw = wide[wi % 3]
                wi += 1
                b.mm(pw[:, 0:512], iqs[:, h, :], ikT[:, g * 1024:g * 1024 + 512])
                b.mm(pw[:, 512:1024], iqs[:, h, :], ikT[:, g * 1024 + 512:(g + 1) * 1024])
                Rr = Rt[h % 2]
                b.act(Rr[:], pw, AF.Relu, scale=wabs[:, h:h + 1])
                if h == 0:
                    b.ts(sc[:, gsl], Rr[:], wsgn[:, 0:1], ALU.mult)
                else:
                    b.stt(sc[:, gsl], Rr[:], wsgn[:, h:h + 1], sc[:, gsl], ALU.mult, ALU.add)
        b.tt(sc[:, S - 1024:S], sc[:, S - 1024:S], capt[:], ALU.min)
        b.memset(lo[:], -1024.0, eng="dve")
        for k in range(33):
            hk = 1024.0 / (2 ** k)
            b.ts(mid[:], lo[:], hk, ALU.add)
            b.op("dve", "tensor_scalar", out=msel[:, 0:S], in0=sc[:, 0:S], scalar1=mid[:, 0:1], scalar2=None,
                 op0=ALU.is_ge, op1=ALU.add, accum_out=cnt[:])
            b.ts(gg[:], cnt[:], 255.5, ALU.is_ge, hk, ALU.mult)
            b.tt(lo[:], lo[:], gg[:], ALU.add)
        b.ts(msel[:, 0:S], sc[:, 0:S], lo[:, 0:1], ALU.is_ge)
        for k0 in range(0, nkt, 8):
            pb = bank_lo()
            v = pbf(pb)
            for k in range(8):
                b.tr(v[:, k * 128:(k + 1) * 128], msel[:, (k0 + k) * 128:(k0 + k + 1) * 128], identb[:])
            b.copy(MT[:, k0:k0 + 8, :], v[:, 0:1024].rearrange("p (k q) -> p k q", q=128), eng="act")
        for rc in range(2):
            for h4 in range(4):
                pb = bank_lo()
                for k in range(4):
                    h = h4 * 4 + k
                    b.mm(pb[:, k * 128:(k + 1) * 128], wukb[:, h, rc * 128:(rc + 1) * 128], qs[:, h, :])
                b.copy(qlT[:, rc, h4 * 512:(h4 + 1) * 512], pb[:, :], eng="act")
        for p in range(4):
            psl = slice(p * 512, (p + 1) * 512)
            def stage1(kt):
                ksl = slice(kt * 128, (kt + 1) * 128)
                pl = bank_lo()
                b.mm(pl[:, :], cT[:, 0, ksl], qlT[:, 0, psl], start=True, stop=False)
                b.mm(pl[:, :], cT[:, 1, ksl], qlT[:, 1, psl], start=False, stop=True)
                E = Et[kt % 3]
                Ef = E[:].rearrange("p h q -> p (h q)")
                b.act(Ef, pl[:, :], AF.Exp, scale=SA_SCALE)
                b.tt(E[:], E[:], MT[:, kt, :].unsqueeze(1).to_broadcast([128, 4, 128]), ALU.mult)
                jj = kt - (8 * m - 8)
                if jj >= 0:
                    ebt = EBt[kt % 2]
                    b.dma("sp", ebt[:], EBn_v[jj, :, p * 4:(p + 1) * 4, :], "eb%d" % (kt % 2))
                    b.tt(E[:], E[:], ebt[:], ALU.mult, eng="pool")
                return Ef

            def stage2(kt, Ef):
                st, sp_ = (kt == 0), (kt == nkt - 1)
                b.mm(po0[:, :], cTM[:, kt, 0:128], Ef, start=st, stop=sp_)
                b.mm(po1[:, :], cTM[:, kt, 128:256], Ef, start=st, stop=sp_)
                b.mm(pdn[:, :], ones_b[:], Ef, start=st, stop=sp_)

            prev = None
            for kt in range(nkt):
                Ef = stage1(kt)
                if prev is not None:
                    stage2(*prev)
                prev = (kt, Ef)
            stage2(*prev)
            b.op("dve", "reciprocal", out=rden[:], in_=pdn[:, :])
            b.tt(olT[:, 0, psl], po0[:, :], rden[:], ALU.mult)
            b.tt(olT[:, 1, psl], po1[:, :], rden[:], ALU.mult)
        for hc in range(8):
            pb = bank_lo()
            i = 0
            for h in (2 * hc, 2 * hc + 1):
                for rc in range(2):
                    b.mm(pb[:, 0:128], wuvb[:, h, rc, :], olT[:, rc, h * 128:(h + 1) * 128], start=(i == 0), stop=(i == 3))
                    i += 1
            b.copy(dsaTs[m % 2][:, hc, :], pb[:, 0:128], eng="act")
        b.dma("sp", dsa_d.rearrange("(c p) t -> p c t", p=128)[:, :, tsl], dsaTs[m % 2][:], "od%d" % (m % 2))
    b.pop()
    if stage == "dsa":
        o = nc.dram_tensor("dsa_out", [128, 8 * NTOK], BF16, kind="ExternalOutput").ap()
        b.dma("sp", o.rearrange("p (c t) -> p c t", c=8), dsa_d.rearrange("(c p) t -> p c t", p=128), "odbg")
    if stage == "dsa":
        b.finish()
        return nc, b

    w3g = IN("w3g", [D, 4096]); wbr = IN("wbr", [1024, D]); wbd = IN("wbd", [1024, D]); wout = IN("wout", [D, D])
    h1_d = DT("h1_d", [D, NTOK])
    b.push()
    xbo = b.sb("xbo2", [128, 16, NTOK], BF16)
    rso = b.sb("rso2", [128, NTOK])
    rwT = b.sb("rwT", [128, 8, NTOK], BF16)
    dsT = b.sb("dsT", [128, 8, NTOK], BF16)
    mT = b.sb("mT", [128, 16, NTOK], BF16)
    wsg = [b.sb("wsg%d" % i, [128, 16, 128]) for i in range(2)]
    wbg = [b.sb("wbg%d" % i, [128, 16, 128], BF16) for i in range(2)]
    gA = b.sb("gA", [128, NTOK]); gB = b.sb("gB", [128, NTOK]); mtmp = b.sb("mtmp", [128, NTOK])
    xres = [b.sb("xres%d" % i, [128, NTOK]) for i in range(2)]
    hch = [b.sb("hch%d" % i, [128, NTOK]) for i in range(2)]
    b.dma("sp", xbo[:], xbo_d.rearrange("(c p) t -> p c t", p=128), "mxa")
    b.dma("sp", rso[:], rso_d, "mxb")
    b.dma("sp", rwT[:], rwo_d.rearrange("(c p) t -> p c t", p=128), "mxc")
    b.dma("sp", dsT[:], dsa_d.rearrange("(c p) t -> p c t", p=128), "mxd")
    wcnt = [0]

    def load_w(view, kc):
        i = wcnt[0] % 2
        wcnt[0] += 1
        b.dma("sp", wsg[i][:, 0:kc, :], view, "wg%d" % i)
        b.copy(wbg[i][:, 0:kc, :], wsg[i][:, 0:kc, :], eng=("act" if i else "pool"))
        return wbg[i]

    w3gv = w3g.rearrange("(c p) n -> p c n", p=128)
    wbrv = wbr.rearrange("(c p) n -> p c n", p=128)
    wbdv = wbd.rearrange("(c p) n -> p c n", p=128)
    woutv = wout.rearrange("(c p) n -> p c n", p=128)
    for dc in range(16):
        cs = slice(dc * 128, (dc + 1) * 128)
        for which, gt in ((0, gA), (1, gB)):
            wb = load_w(w3gv[:, :, which * 2048 + dc * 128: which * 2048 + (dc + 1) * 128], 16)
            for n0 in range(0, NTOK, NCH):
                pb = bank()
                for c in range(16):
                    b.mm(pb[:, 0:NCH], wb[:, c, :], xbo[:, c, n0:n0 + NCH], start=(c == 0), stop=(c == 15))
                b.tt(gt[:, n0:n0 + NCH], pb[:, 0:NCH], rso[:, n0:n0 + NCH], ALU.mult)
            b.act(gt[:], gt[:], AF.Sigmoid)
        wb = load_w(wbrv[:, :, cs], 8)
        for n0 in range(0, NTOK, NCH):
            pb = bank()
            for c in range(8):
                b.mm(pb[:, 0:NCH], wb[:, c, :], rwT[:, c, n0:n0 + NCH], start=(c == 0), stop=(c == 7))
            b.tt(mtmp[:, n0:n0 + NCH], pb[:, 0:NCH], gA[:, n0:n0 + NCH], ALU.mult)
        wb = load_w(wbdv[:, :, cs], 8)
        for n0 in range(0, NTOK, NCH):
            pb = bank()
            for c in range(8):
                b.mm(pb[:, 0:NCH], wb[:, c, :], dsT[:, c, n0:n0 + NCH], start=(c == 0), stop=(c == 7))
            b.tt(gB[:, n0:n0 + NCH], pb[:, 0:NCH], gB[:, n0:n0 + NCH], ALU.mult)
        b.tt(mT[:, dc, :], mtmp[:], gB[:], ALU.add)
    for dc in range(16):
        cs = slice(dc * 128, (dc + 1) * 128)
        wb = load_w(woutv[:, :, cs], 16)
        xr = xres[dc % 2]; hc_ = hch[dc % 2]
        b.dma("sp", xr[:], xTo[cs, :], "xr%d" % (dc % 2))
        for n0 in range(0, NTOK, NCH):
            pb = bank()
            for c in range(16):
                b.mm(pb[:, 0:NCH], wb[:, c, :], mT[:, c, n0:n0 + NCH], start=(c == 0), stop=(c == 15))
            b.tt(hc_[:, n0:n0 + NCH], pb[:, 0:NCH], xr[:, n0:n0 + NCH], ALU.add)
        b.dma("sp", h1_d[cs, :], hc_[:], "oh%d" % (dc % 2))
    if stage in ("mix", "full_dbg"):
        o = nc.dram_tensor("h1_out", [D, NTOK], F32, kind="ExternalOutput").ap()
        b.dma("sp", o, h1_d, "odbg2")
    b.pop()
    if stage == "mix":
        b.finish()
        return nc, b

    gffn = IN("gffn", [128, 16]); wpq = IN("wpq", [D, D]); skT = IN("skT", [128, 16 * 128])
    puT = IN("puT", [D, 16384]); pv = IN("pv", [16384, D]); gfin = IN("gfin", [1, D])
    out_own = nc.dram_tensor("out_own", [NTOK, D], F32, kind="ExternalOutput").ap()
    PH = min(512, NTOK)
    NTL = PH // 128
    gffnt = b.sb("gffnt", [128, 16]); b.dma("sp", gffnt[:], gffn, "c0_11")
    gfbc = b.sb("gfbc", [128, D])
    b.dma("sp", gfbc[:], bass.AP(gfin.tensor, 0, [[0, 128], [1, D]]), "c0_12")
    wpqv = wpq.rearrange("(c p) n -> p c n", p=128)
    puTv = puT.rearrange("(c p) e -> p c e", p=128)
    for ps_ in range(NTOK // PH):
        t0 = ps_ * PH
        b.push()
        hnT = b.sb("hnT", [128, 16, PH], BF16)
        acc = b.sb("acc", [128, NTL, D])
        b.multi[acc[:].tensor.name] = NTL * 4
        S12 = b.sb("S12", [128, NTL, 16, 128])
        tau = b.sb("tau", [128, NTL, 8]); negm = b.sb("negm", [128, NTL, 8])
        e1t = b.sb("e1t", [128, NTL, 8, 128], BF16); e2t = b.sb("e2t", [128, NTL, 8, 128], BF16)
        b.push()
        h1p = b.sb("h1p", [128, 16, PH])
        rs2 = b.sb("rs2", [128, PH])
        q2T = b.sb("q2T", [128, 16, PH])
        skTt = b.sb("skTt", [128, 16, 128]); b.dma("sp", skTt[:], skT.rearrange("p (g n) -> p g n", g=16), "c0_13")
        sqp = q2T[:].rearrange("p g t -> p (g t)").bitcast(BF16)[:, 0:16 * PH].rearrange("p (g t) -> p g t", g=16)
        wsq = [b.sb("wsq%d" % i, [128, 16, 128]) for i in range(2)]
        wbq = [b.sb("wbq%d" % i, [128, 16, 128], BF16) for i in range(2)]
        b.dma("sp", h1p[:], h1_d.rearrange("(c p) t -> p c t", p=128)[:, :, t0:t0 + PH], "x0")
        b.act(sqp, h1p[:], AF.Square)
        pss = bank()
        for c in range(16):
            b.mm(pss[:, 0:PH], ones_b[:], sqp[:, c, :], start=(c == 0), stop=(c == 15))
        b.act(rs2[:], pss[:, 0:PH], AF.Sqrt, scale=1.0 / D, bias=epst[:])
        b.op("dve", "reciprocal", out=rs2[:], in_=rs2[:])
        for c in range(16):
            b.stt(hnT[:, c, :], h1p[:, c, :], gffnt[:, c:c + 1], rs2[:], ALU.mult, ALU.mult)
        for tl in range(NTL):
            for d4 in range(4):
                pb = bank()
                for k in range(4):
                    b.tr(pb[:, k * 128:(k + 1) * 128], h1p[:, d4 * 4 + k, tl * 128:(tl + 1) * 128], ident[:])
                b.copy(acc[:, tl, d4 * 512:(d4 + 1) * 512], pb[:, :], eng="act")
        for g in range(16):
            i = g % 2
            b.dma("sp", wsq[i][:], wpqv[:, :, g * 128:(g + 1) * 128], "wq%d" % i)
            b.copy(wbq[i][:], wsq[i][:], eng=("act" if i else "pool"))
            pb = bank()
            for c in range(16):
                b.mm(pb[:, 0:PH], wbq[i][:, c, :], hnT[:, c, :], start=(c == 0), stop=(c == 15))
            b.copy(q2T[:, g, :], pb[:, 0:PH])
        for tl in range(NTL):
            for g4 in range(4):
                pb = bank()
                for k in range(4):
                    g = g4 * 4 + k
                    b.mm(pb[:, k * 128:(k + 1) * 128], q2T[:, g, tl * 128:(tl + 1) * 128], skTt[:, g, :])
                b.copy(S12[:, tl, g4 * 4:g4 * 4 + 4, :].rearrange("p g n -> p (g n)"), pb[:, :], eng="act")
        b.pop()
        b.push()
        v12 = b.sb("v12", [128, 2, 24]); mrt = b.sb("mrt", [128, 576]); mrt2 = b.sb("mrt2", [128, 576])
        cand = b.sb("cand", [128, 24, 24])
        c16 = b.sb("c16", [128, 16]); e16 = b.sb("e16", [128, 16]); nmx = b.sb("nmx", [128, 1]); zz = b.sb("zz", [128, 1])
        c24 = b.sb("c24", [128, 8]); nm2 = b.sb("nm2", [128, 1])
        for tl in range(NTL):
            for h in range(8):
                for cc in range(2):
                    src = S12[:, tl, 2 * h + cc, :]
                    b.op("dve", "max", out=v12[:, cc, 0:8], in_=src)
                    b.op("dve", "match_replace", out=mrt[:, 0:128], in_to_replace=v12[:, cc, 0:8], in_values=src, imm_value=-1e30)
                    b.op("dve", "max", out=v12[:, cc, 8:16], in_=mrt[:, 0:128])
                    b.op("dve", "match_replace", out=mrt2[:, 0:128], in_to_replace=v12[:, cc, 8:16], in_values=mrt[:, 0:128], imm_value=-1e30)
                    b.op("dve", "max", out=v12[:, cc, 16:24], in_=mrt2[:, 0:128])
                b.tt(cand[:], v12[:, 0, :].unsqueeze(2).to_broadcast([128, 24, 24]),
                     v12[:, 1, :].unsqueeze(1).to_broadcast([128, 24, 24]), ALU.add)
                cf = cand[:].rearrange("p a b -> p (a b)")
                b.op("dve", "max", out=c16[:, 0:8], in_=cf)
                b.op("dve", "match_replace", out=mrt[:], in_to_replace=c16[:, 0:8], in_values=cf, imm_value=-1e30)
                b.op("dve", "max", out=c16[:, 8:16], in_=mrt[:])
                b.op("dve", "match_replace", out=mrt2[:], in_to_replace=c16[:, 8:16], in_values=mrt[:], imm_value=-1e30)
                b.op("dve", "max", out=c24[:], in_=mrt2[:])
                b.ts(tau[:, tl, h:h + 1], c16[:, 15:16], c24[:, 0:1], ALU.add, 0.5, ALU.mult)
                b.ts(nmx[:], c16[:, 0:1], -1.0, ALU.mult)
                b.act(e16[:], c16[:], AF.Exp, bias=nmx[:])
                b.op("dve", "reduce_sum", out=zz[:], in_=e16[:], axis=AX.X)
                b.act(zz[:], zz[:], AF.Ln)
                b.tt(negm[:, tl, h:h + 1], nmx[:], zz[:], ALU.subtract)
                b.ts(nm2[:], v12[:, 1, 0:1], -1.0, ALU.mult)
                b.act(e2t[:, tl, h, :], S12[:, tl, 2 * h + 1, :], AF.Exp, bias=nm2[:])
                b.tt(nm2[:], negm[:, tl, h:h + 1], nm2[:], ALU.subtract)
                b.act(e1t[:, tl, h, :], S12[:, tl, 2 * h, :], AF.Exp, bias=nm2[:])
        b.pop()
        b.push()
        ust = [b.sb("ust%d" % i, [128, 2, 512]) for i in range(2)]
        uTb = b.sb("uTb", [128, 16, 512], BF16)
        vst = [b.sb("vst%d" % i, [128, 1024]) for i in range(2)]
        vbs = [b.sb("vb%d" % i, [128, 4, D], BF16) for i in range(2)]
        GEs = [b.sb("GE%d" % i, [128, 512], BF16) for i in range(NTL)]
        M8s = [b.sb("M8_%d" % i, [128, 8, 4, 128], BF16) for i in range(2)]
        th8s = [b.sb("th8_%d" % i, [128, 8, 4]) for i in range(2)]
        GAs = [b.sb("GA%d" % i, [128, 512], BF16) for i in range(NTL)]
        GATs = [b.sb("GAT%d" % i, [128, 4, 128], BF16) for i in range(2)]

        def load_u(eb):
            for q8 in range(8):
                st = ust[q8 % 2]
                b.dma("sp", st[:], puTv[:, q8 * 2:(q8 + 1) * 2, eb * 512:(eb + 1) * 512], "us%d" % (q8 % 2))
                b.copy(uTb[:, q8 * 2:(q8 + 1) * 2, :], st[:], eng=("act" if q8 % 2 else "pool"))

        def load_v(eb):
            vb = vbs[eb % 2]
            for ec in range(4):
                for hf in range(2):
                    st = vst[hf]
                    b.dma("sp", st[:], pv[eb * 512 + ec * 128: eb * 512 + (ec + 1) * 128, hf * 1024:(hf + 1) * 1024],
                          "vs%d" % hf)
                    b.copy(vb[:, ec, hf * 1024:(hf + 1) * 1024], st[:], eng=("pool" if hf else "act"))

        def gchain(tl, i0):
            M8 = M8s[tl % 2]
            th8 = th8s[tl % 2]
            Sv = S12[:, tl, :, :].rearrange("p (h c) n -> p h c n", c=2)
            b.tt(th8[:], tau[:, tl, :].unsqueeze(2).to_broadcast([128, 8, 4]), Sv[:, :, 0, i0:i0 + 4], ALU.subtract)
            yield
            b.tt(M8[:], Sv[:, :, 1, :].unsqueeze(2).to_broadcast([128, 8, 4, 128]),
                 th8[:].unsqueeze(3).to_broadcast([128, 8, 4, 128]), ALU.is_ge)
            yield
            b.tt(M8[:], M8[:], e2t[:, tl, :, :].unsqueeze(2).to_broadcast([128, 8, 4, 128]), ALU.mult)
            yield
            b.tt(M8[:], M8[:], e1t[:, tl, :, i0:i0 + 4].unsqueeze(3).to_broadcast([128, 8, 4, 128]), ALU.mult)
            yield
            Mf = M8[:].rearrange("p h a j -> p h (a j)")
            b.tt(Mf[:, 0:4, :], Mf[:, 0:4, :], Mf[:, 4:8, :], ALU.add)
            yield
            b.tt(Mf[:, 0:2, :], Mf[:, 0:2, :], Mf[:, 2:4, :], ALU.add)
            yield
            b.tt(Mf[:, 0, :], Mf[:, 0, :], Mf[:, 1, :], ALU.add)
            yield
            b.tt(GAs[tl][:], Mf[:, 0, :], GEs[tl][:], ALU.mult)
            yield

        def gpair(tls, i0):
            gens = [gchain(t_, i0) for t_ in tls]
            while gens:
                for g_ in list(gens):
                    try:
                        next(g_)
                    except StopIteration:
                        gens.remove(g_)

        def stage3(tl, vb):
            pb = bank()
            v = pbf(pb)
            gat = GATs[tl % 2]
            for ec in range(4):
                b.tr(v[:, ec * 128:(ec + 1) * 128], GAs[tl][:, ec * 128:(ec + 1) * 128], identb[:])
            b.copy(gat[:].rearrange("p a t -> p (a t)"), v[:, 0:512], eng="act")
            for d4 in range(4):
                pb2 = bank()
                for ec in range(4):
                    b.mm(pb2[:, :], gat[:, ec, :], vb[:, ec, d4 * 512:(d4 + 1) * 512], start=(ec == 0), stop=(ec == 3))
                b.tt(acc[:, tl, d4 * 512:(d4 + 1) * 512], acc[:, tl, d4 * 512:(d4 + 1) * 512], pb2[:, :], ALU.add)

        load_u(0); load_v(0)
        for eb in range(32):
            i0 = eb * 4
            vb = vbs[eb % 2]
            for tl in range(NTL):
                tsl = slice(tl * 128, (tl + 1) * 128)
                pa = bank()
                for c in range(16):
                    b.mm(pa[:, :], hnT[:, c, tsl], uTb[:, c, :], start=(c == 0), stop=(c == 15))
                b.act(GEs[tl][:], pa[:, :], AF.Gelu)
            if eb + 1 < 32:
                load_u(eb + 1); load_v(eb + 1)
            for t0_ in range(0, NTL, 2):
                pair = list(range(t0_, min(t0_ + 2, NTL)))
                gpair(pair, i0)
                for tl in pair:
                    stage3(tl, vb)
        b.pop()
        b.push()
        sqf = b.sb("sqf", [128, D]); ssf = b.sb("ssf", [128, 1]); of_ = [b.sb("of%d" % i, [128, D]) for i in range(2)]
        for tl in range(NTL):
            b.tt(sqf[:], acc[:, tl, :], acc[:, tl, :], ALU.mult)
            b.op("dve", "reduce_sum", out=ssf[:], in_=sqf[:], axis=AX.X)
            b.act(ssf[:], ssf[:], AF.Sqrt, scale=1.0 / D, bias=epst[:])
            b.op("dve", "reciprocal", out=ssf[:], in_=ssf[:])
            o_ = of_[tl % 2]
            b.stt(o_[:], acc[:, tl, :], ssf[:, 0:1], gfbc[:], ALU.mult, ALU.mult)
            b.dma("sp", out_own[t0 + tl * 128: t0 + (tl + 1) * 128, :], o_[:], "oo%d" % (tl % 2))
        b.pop()
        b.pop()

    b.finish()
    return nc, b


def _rel_bucket_np(rel):
    nb, max_exact = 16, 8
    ret = (rel > 0).astype(np.int32) * nb
    n = np.abs(rel)
    lg = np.log(np.maximum(n, 1).astype(np.float32) / np.float32(max_exact)) / np.float32(math.log(1024 / 8))
    large = max_exact + (lg * np.float32(nb - max_exact)).astype(np.int32)
    large = np.minimum(large, nb - 1)
    return ret + np.where(n < max_exact, n, large)


def own_tokens(c, T):
    ns = T // 1024
    return np.concatenate([np.arange((8 * m + c) * 128, (8 * m + c + 1) * 128) for m in range(ns)])


def host_inputs(inp, T=T, stage="full"):
    f = lambda a: np.ascontiguousarray(np.asarray(a, dtype=np.float32))
    x = f(inp["x"])[0][:T]
    xT = f(x.T)
    w_in = f(inp["w_in"])[0]
    sm = f(inp["shift_mix"])[0]
    RWW = 1024
    o_zw, o_za, o_zg = 3 * RWW, 3 * RWW + 64, 3 * RWW + 128
    o_q = 3360
    o_ckv = o_q + 1024
    o_iq = o_ckv + 256
    o_ik = o_iq + 512
    o_iw = o_ik + 64
    o_g = o_iw + 8
    gm = f(inp["norm_mix_g"])[0]
    shared = {
        "w3q": f(w_in[:, o_q:o_q + 1024]), "w3iq": f(w_in[:, o_iq:o_iq + 512]), "w3iw": f(w_in[:, o_iw:o_iw + 8]),
        "relb": f(inp["rel_bias"]), "relb15": f(f(inp["rel_bias"])[15].reshape(16, 1)),
        "wukT": f(np.transpose(f(inp["w_uk"])[0], (2, 0, 1)).reshape(64, 16 * 256)),
    }
    wuv = f(inp["w_uv"])[0]
    wuvp = np.zeros((128, 16, 2, 128), np.float32)
    for h in range(16):
        for rc in range(2):
            wuvp[:, h, rc, (h % 2) * 64:(h % 2) * 64 + 64] = wuv[h, rc * 128:(rc + 1) * 128, :]
    shared["wuvp"] = f(wuvp.reshape(128, 16 * 2 * 128))
    if stage in ("mix", "full", "full_dbg"):
        shared.update({
            "w3g": f(w_in[:, o_g:o_g + 4096]), "wbr": f(inp["w_branch_rwkv"])[0], "wbd": f(inp["w_branch_dsa"])[0],
            "wout": f(inp["w_out"])[0],
        })
    if stage in ("full", "full_dbg"):
        sk = f(inp["peer_sub_keys"])[0]
        shared.update({
            "gffn": f(f(inp["norm_ffn_g"])[0].reshape(16, 128).T), "wpq": f(inp["w_peer_query"])[0],
            "skT": f(np.transpose(sk, (3, 0, 1, 2)).reshape(128, 16 * 128)),
            "puT": f(f(inp["peer_u"])[0].T), "pv": f(inp["peer_v"])[0],
            "gfin": f(f(inp["norm_final_g"]).reshape(1, D)),
        })
    per_core = []
    for c in range(NCORES):
        cols = []
        for base in (0, RWW, 2 * RWW):
            for h in range(2):
                hh = 2 * c + h
                cols.append(np.arange(base + hh * 64, base + hh * 64 + 64))
        cols.append(np.arange(o_zw, o_zw + 64)); cols.append(np.arange(o_za, o_za + 64))
        cols.append(np.arange(o_zg, o_zg + 160))
        cols.append(np.arange(o_ckv, o_ckv + 256)); cols.append(np.arange(o_ik, o_ik + 64))
        cols = np.concatenate(cols)
        assert cols.size == W1C
        w1 = f(w_in[:, cols])
        pu = np.zeros((64, 2, NPAR), np.float32)
        for h in range(2):
            hh = 2 * c + h
            ch = slice(hh * 64, hh * 64 + 64)
            pu[:, h, P_MIXR] = sm[0 * RWW:][ch]
            pu[:, h, P_MIXK] = sm[1 * RWW:][ch]
            pu[:, h, P_MIXV] = sm[2 * RWW:][ch]
            pu[:, h, P_W0] = f(inp["w0"])[0][ch]
            pu[:, h, P_A0] = f(inp["a0"])[0][ch]
            pu[:, h, P_KK] = f(inp["k_k"])[0][ch]
            pu[:, h, P_KA] = f(inp["k_a"])[0][ch]
            pu[:, h, P_RK] = f(inp["r_k"])[0].reshape(-1)[ch]
            pu[:, h, P_LNW] = f(inp["ln_x_w"])[0][ch]
            pu[:, h, P_LNB] = f(inp["ln_x_b"])[0][ch]
        mixl = np.zeros((64, 5), np.float32)
        mixl[:, 0] = sm[o_zw:o_zw + 64]; mixl[:, 1] = sm[o_za:o_za + 64]
        mixl[:, 2] = sm[o_zg:o_zg + 64]; mixl[:, 3] = sm[o_zg + 64:o_zg + 128]
        mixl[:32, 4] = sm[o_zg + 128:o_zg + 160]
        own = slice(c * 128, (c + 1) * 128)
        wlow = np.zeros((64, 5, 128), np.float32)
        wlow[:, 0] = f(inp["w_decay_up"])[0][:, own]
        wlow[:, 1] = f(inp["w_icl_up"])[0][:, own]
        wg = f(inp["w_gate_up"])[0][:, own]
        wlow[:, 2] = wg[0:64]; wlow[:, 3] = wg[64:128]; wlow[:32, 4] = wg[128:160]
        d = {
            "xT": xT, "w1": w1, "gmix": f(gm.reshape(16, 128).T),
            "pu": f(pu.reshape(64, 2 * NPAR)), "mixl": mixl, "wlow": f(wlow.reshape(64, 5 * 128)),
            "kvg": f(f(inp["kv_norm_g"])[0].reshape(2, 128).T),
        }
        if stage != "rwkv":
            toks = own_tokens(c, T)
            d["sel"] = f(np.tile((np.arange(8) == c).astype(np.float32)[None, :], (128, 1)))
            d["xTo"] = f(xT[:, toks])
            cap = np.full((128, 8, 128), -1e30, np.float32)
            qi = np.arange(128)[:, None]; si = np.arange(128)[None, :]
            for j in range(8):
                if j < c:
                    cap[:, j, :] = 3e38
                elif j == c:
                    cap[:, j, :] = np.where((si >= 64) & (qi < 64), -1e30, 3e38)
            d["cap"] = f(cap.reshape(128, 1024))
            xs_ = np.arange(NVEC)
            bk = _rel_bucket_np(xs_ - 127 - 128 * (8 + c))
            d["ohc"] = f((bk[None, :] == np.arange(32)[:, None]).astype(np.float32))
            d.update(shared)
        per_core.append(d)
    return per_core


def kernel(**inputs):
    nc, b = build("full")
    in_maps = host_inputs(inputs, T=T, stage="full")
    res = run_bass_kernel_spmd(nc, in_maps, core_ids=list(range(NCORES)))
    out = np.zeros((1, T, D), np.float32)
    for c in range(NCORES):
        out[0, own_tokens(c, T)] = res.results[c]["out_own"]
    return out
```
